# Optimizing a Trainium2 kernel written in Bass

```python
import jax, jax.numpy as jnp
from jax import lax
import numpy as np

D_MODEL = 1024
BATCH = 8
SEQ = 2048
DEPTH = 2

GRID_W = 64
CTX_LEN = 256

NA_HEADS = 32
NA_HEAD_DIM = D_MODEL // NA_HEADS
WIN_R = 8
WIN_C = 16
QBLK_C = 16
KBLK_C = QBLK_C + WIN_C
NEG_INF = -1e30

RET_HEADS = 4
RET_QK_DIM = D_MODEL // RET_HEADS
RET_V_DIM = 2 * RET_QK_DIM
RET_DQK = RET_HEADS * RET_QK_DIM
RET_DV = RET_HEADS * RET_V_DIM
RET_CHUNK = 128

N_EXPERTS = 16
N_GROUPS = 4
EXPERTS_PER_GROUP = N_EXPERTS // N_GROUPS
TOP_K = 2
D_EXPERT = 512

N_MOD = 6
EPS = 1e-6
N_NA_LAYERS = (DEPTH + 1) // 2
N_RET_LAYERS = DEPTH // 2

kernel_name = 'hybrid_natten_retention_groupmoe_dit'


def rmsnorm(x, g):
    xf = x.astype(jnp.float32)
    xf = xf * lax.rsqrt(jnp.mean(xf * xf, axis=-1, keepdims=True) + EPS)
    return xf.astype(x.dtype) * g


def modulate(xn, shift, scale):
    return xn * (1 + scale) + shift


def split_heads(t, n_heads):
    b, l, _ = t.shape
    return t.reshape(b, l, n_heads, -1).transpose(0, 2, 1, 3)


def merge_heads(t):
    b, h, l, d = t.shape
    return t.transpose(0, 2, 1, 3).reshape(b, l, h * d)


def _na_col_tables():
    qs = np.arange(GRID_W // QBLK_C) * QBLK_C
    kb = np.clip(qs - WIN_C // 2, 0, GRID_W - KBLK_C)
    kcols = kb[:, None] + np.arange(KBLK_C)
    qcols = qs[:, None] + np.arange(QBLK_C)
    qstart = np.clip(qcols - WIN_C // 2, 0, GRID_W - WIN_C)
    kc = kcols[:, None, :]
    st = qstart[:, :, None]
    valid = (kc >= st) & (kc < st + WIN_C)
    dx = np.clip(kc - qcols[:, :, None] + WIN_C - 1, 0, 2 * WIN_C - 2)
    return kcols, valid, dx


def neighbourhood_attention(h, hc, w_in, w_out, rpb, with_ctx_out):
    b, l, _ = h.shape
    rows = l // GRID_W
    wr = min(WIN_R, rows)
    ncb = GRID_W // QBLK_C
    nk = wr * KBLK_C
    scale = NA_HEAD_DIM ** -0.5
    q, k, v = jnp.split(h @ w_in, 3, axis=-1)
    q, k, v = split_heads(q * scale, NA_HEADS), split_heads(k, NA_HEADS), split_heads(v, NA_HEADS)
    qc, kc, vc = jnp.split(hc @ w_in, 3, axis=-1)
    qc, kc, vc = split_heads(qc * scale, NA_HEADS), split_heads(kc, NA_HEADS), split_heads(vc, NA_HEADS)
    grid = lambda t: t.reshape(b, NA_HEADS, rows, GRID_W, NA_HEAD_DIM)
    qg, kg, vg = grid(q), grid(k), grid(v)
    kcols, valid, dx = _na_col_tables()
    valid = jnp.asarray(np.repeat(valid[:, :, None, :], wr, axis=2).reshape(ncb, QBLK_C, nk))
    dx = jnp.asarray(dx)

    def row_block(r):
        r0 = jnp.clip(r - wr // 2, 0, rows - wr)
        dy = r0 + jnp.arange(wr) - r + (WIN_R - 1)
        bias = rpb[:, dy[None, None, :, None], dx[:, :, None, :]]
        bias = bias.reshape(NA_HEADS, ncb, QBLK_C, nk).astype(jnp.float32)

        def gather(t):
            t = lax.dynamic_slice_in_dim(t, r0, wr, axis=2)[:, :, :, kcols]
            return t.transpose(0, 1, 3, 2, 4, 5).reshape(b, NA_HEADS, ncb, nk, NA_HEAD_DIM)

        kb, vb = gather(kg), gather(vg)
        qr = lax.dynamic_index_in_dim(qg, r, axis=2, keepdims=False)
        qr = qr.reshape(b, NA_HEADS, ncb, QBLK_C, NA_HEAD_DIM)
        s_loc = jnp.einsum('bhnqd,bhnkd->bhnqk', qr, kb).astype(jnp.float32) + bias
        s_loc = jnp.where(valid, s_loc, NEG_INF)
        s_ctx = jnp.einsum('bhnqd,bhcd->bhnqc', qr, kc).astype(jnp.float32)
        p = jax.nn.softmax(jnp.concatenate([s_loc, s_ctx], axis=-1), axis=-1).astype(vb.dtype)
        o = (jnp.einsum('bhnqk,bhnkd->bhnqd', p[..., :nk], vb)
             + jnp.einsum('bhnqc,bhcd->bhnqd', p[..., nk:], vc))
        return o.reshape(b, NA_HEADS, GRID_W, NA_HEAD_DIM)

    o = lax.map(row_block, jnp.arange(rows))
    o = o.transpose(1, 0, 3, 2, 4).reshape(b, l, D_MODEL)
    y = o @ w_out
    yc = None
    if with_ctx_out:
        s = jnp.einsum('bhqd,bhkd->bhqk', qc, kc).astype(jnp.float32)
        p = jax.nn.softmax(s, axis=-1).astype(vc.dtype)
        yc = merge_heads(jnp.einsum('bhqk,bhkd->bhqd', p, vc)) @ w_out
    return y, yc


def _rotary(t, theta):
    ang = jnp.arange(t.shape[2], dtype=jnp.float32)[:, None] * theta[None, :]
    cos, sin = jnp.cos(ang), jnp.sin(ang)
    t1, t2 = jnp.split(t, 2, axis=-1)
    return jnp.concatenate([t1 * cos - t2 * sin, t1 * sin + t2 * cos], axis=-1)


def chunk_retention(q, k, v, log_gamma, state0):
    b, h, l, _ = q.shape
    dv = v.shape[-1]
    nc = l // RET_CHUNK
    idx = jnp.arange(RET_CHUNK, dtype=jnp.float32)
    diff = idx[:, None] - idx[None, :]
    lg = log_gamma[:, None, None]
    decay = jnp.where(diff >= 0, jnp.exp(jnp.maximum(diff, 0.0) * lg), 0.0)
    into = jnp.exp((idx + 1.0)[None, :] * log_gamma[:, None])[..., None]
    out_of = jnp.exp((RET_CHUNK - 1.0 - idx)[None, :] * log_gamma[:, None])[..., None]
    chunk_decay = jnp.exp(RET_CHUNK * log_gamma)[:, None, None]
    to_chunks = lambda t: t.reshape(b, h, nc, RET_CHUNK, t.shape[-1]).transpose(2, 0, 1, 3, 4)

    def step(state, qkv):
        qc, kc, vc = qkv
        intra = jnp.einsum('bhij,bhjd->bhid', jnp.einsum('bhid,bhjd->bhij', qc, kc) * decay, vc)
        cross = jnp.einsum('bhid,bhde->bhie', qc * into, state)
        state = state * chunk_decay + jnp.einsum('bhjd,bhje->bhde', kc * out_of, vc)
        return state, intra + cross

    state, o = lax.scan(step, state0, (to_chunks(q), to_chunks(k), to_chunks(v)))
    return state, o.transpose(1, 2, 0, 3, 4).reshape(b, h, l, dv)


def retention_mixer(h, hc, w_in, w_out, logit_fwd, logit_bwd, g_norm, with_ctx_out):
    b = h.shape[0]
    dt = h.dtype
    f32 = jnp.float32

    def project(t):
        q, k, v, g = jnp.split(t @ w_in, [RET_DQK, 2 * RET_DQK, 2 * RET_DQK + RET_DV], axis=-1)
        q = split_heads(q, RET_HEADS).astype(f32)
        k = split_heads(k, RET_HEADS).astype(f32) * (RET_QK_DIM ** -0.5)
        v = split_heads(v, RET_HEADS).astype(f32)
        return q, k, v, g

    q, k, v, g = project(h)
    qc, kc, vc, gc = project(hc)
    theta = 1.0 / (10000.0 ** jnp.linspace(0.0, 1.0, RET_QK_DIM // 2, dtype=f32))
    q, k = _rotary(q, theta), _rotary(k, theta)
    lgf = jax.nn.log_sigmoid(logit_fwd.astype(f32))
    lgb = jax.nn.log_sigmoid(logit_bwd.astype(f32))
    zeros = jnp.zeros((b, RET_HEADS, RET_QK_DIM, RET_V_DIM), f32)
    flip = lambda t: jnp.flip(t, axis=2)
    s_fwd, ycf = chunk_retention(qc, kc, vc, lgf, zeros)
    s_bwd, ycb = chunk_retention(flip(qc), flip(kc), flip(vc), lgb, zeros)
    _, yf = chunk_retention(q, k, v, lgf, s_fwd)
    _, yb = chunk_retention(flip(q), flip(k), flip(v), lgb, s_bwd)

    def output(o, gate):
        o = o * lax.rsqrt(jnp.mean(o * o, axis=-1, keepdims=True) + EPS)
        o = merge_heads(o).astype(dt) * g_norm
        return (jax.nn.silu(gate) * o) @ w_out

    y = output(yf + flip(yb), g)
    yc = output(ycf + flip(ycb), gc) if with_ctx_out else None
    return y, yc


def grouped_moe(h, router_w, router_b, w_gate, w_up, w_down):
    shp = h.shape
    t = h.reshape(-1, shp[-1])
    scores = jax.nn.sigmoid((t @ router_w).astype(jnp.float32))
    sel = scores + router_b.astype(jnp.float32)
    grouped = sel.reshape(-1, N_GROUPS, EXPERTS_PER_GROUP)
    group_score = lax.top_k(grouped, TOP_K)[0].sum(-1)
    g_idx = jnp.argmax(group_score, axis=-1)
    in_group = jnp.take_along_axis(grouped, g_idx[:, None, None], axis=1)[:, 0]
    _, loc = lax.top_k(in_group, TOP_K)
    e_idx = g_idx[:, None] * EXPERTS_PER_GROUP + loc
    w = jnp.take_along_axis(scores, e_idx, axis=1)
    w = w / jnp.sum(w, axis=-1, keepdims=True)
    gates = jnp.einsum('tk,tke->te', w, jax.nn.one_hot(e_idx, N_EXPERTS, dtype=jnp.float32)).astype(h.dtype)
    out = jnp.zeros_like(t)
    for e in range(N_EXPERTS):
        hid = jax.nn.silu(t @ w_gate[e]) * (t @ w_up[e])
        out = out + gates[:, e:e + 1] * (hid @ w_down[e])
    return out.reshape(shp)


def setup_inputs(seed: int = 0) -> dict:
    key = jax.random.key(seed)
    ks = jax.random.split(key, 22)
    nrm = jax.random.normal
    f32 = jnp.float32
    base_logit = jnp.log(2.0 ** (5.0 + jnp.arange(RET_HEADS, dtype=f32)) - 1.0)
    return {
        'x': nrm(ks[0], (BATCH, SEQ, D_MODEL), f32),
        'c': nrm(ks[1], (BATCH, D_MODEL), f32),
        'ctx': nrm(ks[2], (BATCH, CTX_LEN, D_MODEL), f32),
        'c_ctx': nrm(ks[3], (D_MODEL,), f32),
        'w_ada': nrm(ks[4], (DEPTH, D_MODEL, N_MOD * D_MODEL), f32) * (0.5 * D_MODEL ** -0.5),
        'b_ada': nrm(ks[5], (DEPTH, N_MOD * D_MODEL), f32) * 0.02,
        'norm_mix': 1.0 + 0.02 * nrm(ks[6], (DEPTH, D_MODEL), f32),
        'norm_ffn': 1.0 + 0.02 * nrm(ks[7], (DEPTH, D_MODEL), f32),
        'na_w_in': nrm(ks[8], (N_NA_LAYERS, D_MODEL, 3 * D_MODEL), f32) * D_MODEL ** -0.5,
        'na_w_out': nrm(ks[9], (N_NA_LAYERS, D_MODEL, D_MODEL), f32) * D_MODEL ** -0.5,
        'na_rpb': nrm(ks[10], (N_NA_LAYERS, NA_HEADS, 2 * WIN_R - 1, 2 * WIN_C - 1), f32) * 0.02,
        'ret_w_in': nrm(ks[11], (N_RET_LAYERS, D_MODEL, 2 * RET_DQK + 2 * RET_DV), f32) * D_MODEL ** -0.5,
        'ret_w_out': nrm(ks[12], (N_RET_LAYERS, RET_DV, D_MODEL), f32) * RET_DV ** -0.5,
        'ret_decay_fwd': base_logit + 0.1 * nrm(ks[13], (N_RET_LAYERS, RET_HEADS), f32),
        'ret_decay_bwd': base_logit + 0.1 * nrm(ks[14], (N_RET_LAYERS, RET_HEADS), f32),
        'ret_norm': 1.0 + 0.02 * nrm(ks[15], (N_RET_LAYERS, RET_DV), f32),
        'router_w': nrm(ks[16], (D_MODEL, N_EXPERTS), f32) * D_MODEL ** -0.5,
        'router_b': nrm(ks[17], (N_EXPERTS,), f32) * 0.01,
        'moe_w_gate': nrm(ks[18], (DEPTH, N_EXPERTS, D_MODEL, D_EXPERT), f32) * D_MODEL ** -0.5,
        'moe_w_up': nrm(ks[19], (DEPTH, N_EXPERTS, D_MODEL, D_EXPERT), f32) * D_MODEL ** -0.5,
        'moe_w_down': nrm(ks[20], (DEPTH, N_EXPERTS, D_EXPERT, D_MODEL), f32) * D_EXPERT ** -0.5,
        'final_norm': 1.0 + 0.02 * nrm(ks[21], (D_MODEL,), f32),
    }


def reference(x, c, ctx, c_ctx, w_ada, b_ada, norm_mix, norm_ffn, na_w_in, na_w_out, na_rpb,
              ret_w_in, ret_w_out, ret_decay_fwd, ret_decay_bwd, ret_norm,
              router_w, router_b, moe_w_gate, moe_w_up, moe_w_down, final_norm):
    seq = x.shape[1]
    sc = jax.nn.silu(c)
    scc = jax.nn.silu(c_ctx)
    for i in range(DEPTH):
        last = i == DEPTH - 1
        j = i // 2
        sh1, s1, g1, sh2, s2, g2 = [m[:, None, :] for m in jnp.split(sc @ w_ada[i] + b_ada[i], N_MOD, axis=-1)]
        csh1, cs1, cg1, csh2, cs2, cg2 = jnp.split(scc @ w_ada[i] + b_ada[i], N_MOD, axis=-1)
        h = modulate(rmsnorm(x, norm_mix[i]), sh1, s1)
        hc = modulate(rmsnorm(ctx, norm_mix[i]), csh1, cs1)
        if i % 2 == 0:
            y, yc = neighbourhood_attention(h, hc, na_w_in[j], na_w_out[j], na_rpb[j], not last)
        else:
            y, yc = retention_mixer(h, hc, ret_w_in[j], ret_w_out[j], ret_decay_fwd[j],
                                    ret_decay_bwd[j], ret_norm[j], not last)
        x = x + g1 * y
        if last:
            hx = modulate(rmsnorm(x, norm_ffn[i]), sh2, s2)
            x = x + g2 * grouped_moe(hx, router_w, router_b, moe_w_gate[i], moe_w_up[i], moe_w_down[i])
        else:
            ctx = ctx + cg1 * yc
            hx = modulate(rmsnorm(x, norm_ffn[i]), sh2, s2)
            hcx = modulate(rmsnorm(ctx, norm_ffn[i]), csh2, cs2)
            f = grouped_moe(jnp.concatenate([hx, hcx], axis=1), router_w, router_b,
                            moe_w_gate[i], moe_w_up[i], moe_w_down[i])
            x = x + g2 * f[:, :seq]
            ctx = ctx + cg2 * f[:, seq:]
    return rmsnorm(x, final_norm)
```

```python
import numpy as np
import concourse.bass as bass
import concourse.mybir as mybir
from concourse.bass_utils import run_bass_kernel_spmd

F32 = mybir.dt.float32
BF16 = mybir.dt.bfloat16
AF = mybir.ActivationFunctionType
ALU = mybir.AluOpType
AX = mybir.AxisListType

D = 1024
T = 2048
C = 256
TT = T + C
EPS = 1e-6
SEM_CH = 16000


class Buf:
    __slots__ = ("name", "w", "rs", "dsem", "dcount")

    def __init__(self, name):
        self.name = name
        self.w = None
        self.rs = []
        self.dsem = None
        self.dcount = 0


class Eng:
    def __init__(self, name):
        self.name = name
        self.count = 0
        self.sems = []
        self.seen = {}
        self.seen_d = {}
        self.ops = []


class _Rec:
    def __init__(self):
        self.call = None

    def __getattr__(self, name):
        def f(*a, **k):
            assert self.call is None
            self.call = (name, a, k)
            return None
        return f


class Prog:
    ENGS = ("pe", "act", "dve", "pool", "sp")

    def __init__(self, nc):
        self.nc = nc
        self.eng = {n: Eng(n) for n in self.ENGS}
        self.nsem = 0
        self.dbufs = []

    def new_sem(self, name):
        self.nsem += 1
        return self.nc.semaphore(f"s{self.nsem}_{name}").__enter__()

    def _sem_for(self, E, m):
        k = (m - 1) // SEM_CH
        while len(E.sems) <= k:
            E.sems.append(self.new_sem(f"{E.name}{len(E.sems)}"))
        return E.sems[k], (m - 1) % SEM_CH + 1

    def _need(self, F, tok, waits):
        if tok is None:
            return
        if tok[0] == "E":
            _, en, m = tok
            if F.seen.get(en, 0) >= m:
                return
            F.seen[en] = m
            waits.append(self._sem_for(self.eng[en], m))
        else:
            _, b, c = tok
            if F.seen_d.get(id(b), 0) >= c:
                return
            F.seen_d[id(b)] = c
            waits.append((b.dsem, c))

    def _deps(self, F, reads, writes, is_dma=False):
        waits = []
        for b in reads:
            t = b.w
            if t is None:
                continue
            if t[0] == "E" and t[1] == F.name and F.name == "pe" and not is_dma:
                continue
            self._need(F, t, waits)
        for b in writes:
            for t in [b.w] + b.rs:
                if t is None:
                    continue
                if t[0] == "E" and t[1] == F.name and F.name == "pe" and not is_dma:
                    continue
                self._need(F, t, waits)
        return waits

    def op(self, en, fn, reads=(), writes=(), inc=True, lowp=None):
        rec = _Rec()
        fn(rec)
        name_, a_, k_ = rec.call
        nc_ = self.nc

        def fn(e, name_=name_, a_=a_, k_=k_):
            if lowp is not None:
                with nc_.allow_low_precision(reason=lowp):
                    return getattr(e, name_)(*a_, **k_)
            return getattr(e, name_)(*a_, **k_)
        F = self.eng[en]
        waits = self._deps(F, reads, writes)
        m = F.count + 1
        tok = ("E", en, m)
        if inc:
            F.count = m
            sem, _ = self._sem_for(F, m)
            F.ops.append((waits, fn, (sem, 1)))
        else:
            F.ops.append((waits, fn, None))
        for b in reads:
            b.rs.append(tok)
        for b in writes:
            b.w = tok
            b.rs = []
        return tok

    def dma(self, q, out, in_, reads=(), writes=(), **kw):
        F = self.eng[q]
        waits = self._deps(F, reads, writes, is_dma=True)
        wb = writes[0]
        if wb.dsem is None:
            wb.dsem = self.new_sem("d_" + wb.name)
            self.dbufs.append(wb)
        wb.dcount += 16
        tok = ("D", wb, wb.dcount)
        sem = wb.dsem

        def fn(e, out=out, in_=in_, kw=kw):
            return e.dma_start(out=out, in_=in_, **kw)
        F.ops.append((waits, fn, (sem, 16)))
        for b in reads:
            b.rs.append(tok)
        for b in writes:
            b.w = tok
            b.rs = []
        return tok

    def barrier(self):
        for F in self.eng.values():
            waits = []
            for E in self.eng.values():
                if E is not F and E.count > 0:
                    self._need(F, ("E", E.name, E.count), waits)
            for b in self.dbufs:
                if b.dcount:
                    self._need(F, ("D", b, b.dcount), waits)
            F.ops.append((waits, None, None))

    def emit(self):
        nc = self.nc

        def replay(e, ops):
            for waits, fn, inc in ops:
                for sem, val in waits:
                    e.wait_ge(sem, val)
                if fn is None:
                    continue
                ins = fn(e)
                if inc is not None:
                    ins.then_inc(inc[0], inc[1])

        with nc.Block() as block:
            @block.tensor
            def _(e):
                replay(e, self.eng["pe"].ops)

            @block.scalar
            def _(e):
                replay(e, self.eng["act"].ops)

            @block.vector
            def _(e):
                replay(e, self.eng["dve"].ops)

            @block.gpsimd
            def _(e):
                replay(e, self.eng["pool"].ops)

            @block.sync
            def _(e):
                replay(e, self.eng["sp"].ops)


class RR:
    def __init__(self, items):
        self.items = list(items)
        self.i = 0

    def next(self):
        v = self.items[self.i % len(self.items)]
        self.i += 1
        return v


NV = 152
V_CV, V_BADA, V_NMIX, V_NFFN, V_FN = 0, 16, 112, 128, 144


def _fm(v):
    return np.ascontiguousarray(v.reshape(-1, 128).T)


def _na_table(rpb):
    H = rpb.shape[0]
    kc = np.arange(64)[:, None]
    qc = np.arange(64)[None, :]
    qstart = np.clip(qc - 8, 0, 48)
    valid = (kc >= qstart) & (kc < qstart + 16)
    dx = np.clip(kc - qc + 15, 0, 30)
    tab = np.empty((2, 64, H, 14, 64), np.float32)
    for j2 in range(2):
        for e in range(14):
            g = rpb[:, e + j2, :][:, dx]
            g = np.where(valid[None], g, np.float32(-1e30))
            tab[j2, :, :, e, :] = g.transpose(1, 0, 2)
    return tab.reshape(128, H * 14 * 64)


def _ret_consts():
    j = np.arange(128, dtype=np.float32)[:, None]
    i = np.arange(128, dtype=np.float32)[None, :]
    r = np.zeros((128, 128 * 6 + 4), np.float32)
    r[:, 0:128] = np.maximum(i - j, 0)
    r[:, 128:256] = np.maximum(j - i, 0)
    r[:, 256:384] = (i >= j) / 16.0
    r[:, 384:512] = (j >= i) / 16.0
    r[:, 512:640] = i + 1.0
    r[:, 640:768] = 128.0 - i
    r[:, 768] = 127.0 - j[:, 0]
    r[:, 769] = j[:, 0]
    r[:, 770] = 128.0
    r[:, 771] = 1.0 / 16.0
    return r


def _rot_tables():
    theta = (1.0 / (10000.0 ** np.linspace(0.0, 1.0, 128, dtype=np.float32))).astype(np.float32)
    n = np.arange(T, dtype=np.float32)
    ang = (n[None, :] * theta[:, None]).astype(np.float32)
    return np.cos(ang.astype(np.float64)).astype(np.float32), np.sin(ang.astype(np.float64)).astype(np.float32)


def build(stop=99, debug=False):
    nc = bass.Bass("TRN2", target_bir_lowering=False)
    P = Prog(nc)

    def din(name, shape):
        return nc.dram_tensor(name, list(shape), F32, kind="ExternalInput")

    x_d = din("x", [T, D]).ap()
    ctx_d = din("ctx", [C, D]).ap()
    vecs_d = din("vecs", [128, NV]).ap()
    w_ada_d = din("w_ada", [2, D, 6 * D]).ap()
    na_w_in_d = din("na_w_in", [D, 3 * D]).ap()
    na_w_out_d = din("na_w_out", [D, D]).ap()
    natab_d = din("natab", [128, 32 * 14 * 64]).ap()
    ret_w_in_d = din("ret_w_in", [D, 6 * D]).ap()
    ret_w_out_d = din("ret_w_out", [2 * D, D]).ap()
    ret_dec_h = din("ret_dec", [1, 8])
    ret_norm_h = din("ret_norm", [1, 2 * D])
    rcst_d = din("rcst", [128, 772]).ap()
    cos_d = din("rcos", [128, T]).ap()
    sin_d = din("rsin", [128, T]).ap()
    router_w_d = din("router_w", [D, 16]).ap()
    router_b_h = din("router_b", [1, 16])
    wg_d = din("moe_w_gate", [2, 16, D, 512]).ap()
    wu_d = din("moe_w_up", [2, 16, D, 512]).ap()
    wd_d = din("moe_w_down", [2, 16, 512, D]).ap()
    ident_d = din("ident", [128, 128]).ap()
    sel_d = din("sel", [16, 16 * 128]).ap()
    out_d = nc.dram_tensor("out", [T, D], F32, kind="ExternalOutput").ap()
    qT_s = nc.dram_tensor("qT_s", [D, TT], BF16).ap()
    kT_s = nc.dram_tensor("kT_s", [D, TT], BF16).ap()
    v_s = nc.dram_tensor("v_s", [TT * 2 * D], BF16).ap()
    sg_s = nc.dram_tensor("sg_s", [T, 2 * D], BF16).ap()
    qT_b, kT_b, v_b, sg_b, out_b = Buf("qT_s"), Buf("kT_s"), Buf("v_s"), Buf("sg_s"), Buf("out")
    dbg = {}

    AB = 208000
    arena = nc.alloc_sbuf_tensor("arena", [128, AB // 4], F32)
    ps = nc.alloc_psum_tensor("ps", [128, 4096], F32)
    psb = [Buf(f"ps{i}") for i in range(8)]

    def bank(i):
        return ps[:, 512 * i:512 * (i + 1)]

    def bankbf(i):
        return ps[:, 512 * i:512 * (i + 1)].bitcast(BF16)

    def view(off, nbytes, dt=F32):
        assert off % 4 == 0 and nbytes % 4 == 0 and off + nbytes <= AB, (off, nbytes)
        a_ = arena[:, off // 4:(off + nbytes) // 4]
        return a_ if dt == F32 else a_.bitcast(dt)

    XT = view(0, 65536).rearrange("p (c n) -> p c n", c=8)
    CT = view(65536, 8192).rearrange("p (c n) -> p c n", c=8)
    HT = view(73728, 36864, BF16).rearrange("p (c n) -> p c n", c=8)
    HT_raw_off = 73728
    pp = [110592]

    def palloc(nbytes, dt=F32):
        o = pp[0]
        pp[0] += (nbytes + 31) // 32 * 32
        return view(o, nbytes, dt)

    ident = palloc(256, BF16)
    ones = palloc(256, BF16)
    vecs = palloc(NV * 4)
    modT = [palloc(2 * 48 * 4).rearrange("p (t f) -> p t f", t=2) for _ in range(2)]
    der = [palloc(6 * 2 * 8 * 4).rearrange("p (k t c) -> p k t c", k=6, t=2) for _ in range(2)]
    nS = palloc(40 * 4)
    epsc = palloc(32)
    scb = palloc(8 * 2 * 2, BF16).rearrange("p (c t) -> p c t", t=2)
    rwb = palloc(8 * 16 * 2, BF16).rearrange("p (c e) -> p c e", e=16)
    rb18 = palloc(18 * 16 * 4).rearrange("p (t e) -> p t e", e=16)
    ARENA0 = (pp[0] + 63) // 64 * 64
    assert ARENA0 <= 119808, ARENA0
    b_ident, b_ones, b_vecs, b_mod, b_der, b_nS, b_eps, b_scb, b_rwb, b_rb18 = (Buf(n) for n in
        ("ident", "ones", "vecs", "mod", "der", "nS", "eps", "scb", "rwb", "rb18"))
    XB = [Buf(f"xt{i}") for i in range(4)] + [Buf("ct")]
    HB = [Buf(f"ht{i}") for i in range(5)]
    BLK = [(i * 512, 512) for i in range(4)] + [(2048, 256)]

    def xt_blk(b):
        if b < 4:
            return XT[:, :, b * 512:(b + 1) * 512]
        return CT[:, :, :]

    class Arena:
        def __init__(self):
            self.o = ARENA0

        def alloc(self, nbytes, dt=F32):
            o = self.o
            self.o += (nbytes + 63) // 64 * 64
            assert self.o <= AB, ("arena overflow", self.o)
            return view(o, nbytes, dt)

    def mm(out, lhsT, rhs, start, stop, reads, writes, inc, **kw):
        P.op("pe", lambda e: e.matmul(out, lhsT=lhsT, rhs=rhs, start=start, stop=stop, **kw), reads, writes, inc=inc)

    evac_rr = RR(["act", "dve"])

    def copy_any(eng, out, in_, reads, writes, scale=None):
        if eng == "act":
            if scale is None:
                P.op("act", lambda e: e.activation(out=out, in_=in_, func=AF.Copy), reads, writes)
            else:
                P.op("act", lambda e: e.activation(out=out, in_=in_, func=AF.Identity, scale=scale), reads, writes)
        else:
            if scale is None:
                P.op(eng, lambda e: e.tensor_copy(out=out, in_=in_), reads, writes)
            else:
                P.op(eng, lambda e: e.tensor_scalar(out=out, in0=in_, scalar1=scale, scalar2=None, op0=ALU.mult), reads, writes)

    def load_piece(dst, dstb, src_rows_cols):
        P.dma("pool", dst, src_rows_cols.rearrange("(c p) n -> p c n", p=128), writes=[dstb])

    def norm_mod(A, b0, b1, kA, kS, layer, dst_is_final=False):
        sq = [A.alloc(8 * 512 * 2, BF16).rearrange("p (c n) -> p c n", c=8) for _ in range(2)]
        rst = [A.alloc(512 * 4) for _ in range(2)]
        NT = 8
        tmp = [A.alloc(512 * 4) for _ in range(NT)]
        b_sq = [[Buf(f"sq{i}_{c}") for c in range(8)] for i in range(2)]
        b_rst = [Buf("rst0"), Buf("rst1")]
        b_tmp = [Buf(f"nt{i}") for i in range(NT)]
        kk = [0]

        def stats(b):
            t0, n = BLK[b]
            i = b % 2
            src = xt_blk(b)
            for c in range(8):
                P.op("act", lambda e: e.activation(out=sq[i][:, c, :n], in_=src[:, c, :], func=AF.Square),
                     [XB[b]], [b_sq[i][c]])
                mm(bank(0)[:, :n], ones, sq[i][:, c, :n], c == 0, c == 7, [b_ones, b_sq[i][c]], [psb[0]], c == 7)

        def recip(b):
            t0, n = BLK[b]
            i = b % 2
            P.op("act", lambda e: e.activation(out=rst[i][:, :n], in_=bank(0)[:, :n], func=AF.Sqrt, bias=epsc[:, 0:1]),
                 [psb[0], b_eps], [b_rst[i]])
            P.op("dve", lambda e: e.reciprocal(out=rst[i][:, :n], in_=rst[i][:, :n]), [b_rst[i]], [b_rst[i]])

        def apply(b):
            t0, n = BLK[b]
            t = 0 if b < 4 else 1
            i = b % 2
            src = xt_blk(b)
            for c in range(8):
                tb = kk[0] % NT
                kk[0] += 1
                P.op("dve", lambda e: e.scalar_tensor_tensor(
                    out=tmp[tb][:, :n], in0=src[:, c, :], scalar=der[layer][:, kA, t, c:c + 1], in1=rst[i][:, :n],
                    op0=ALU.mult, op1=ALU.mult), [XB[b], b_der, b_rst[i]], [b_tmp[tb]])
                P.op("act", lambda e: e.activation(
                    out=HT[:, c, t0:t0 + n], in_=tmp[tb][:, :n], func=AF.Identity, bias=der[layer][:, kS, t, c:c + 1]),
                    [b_tmp[tb], b_der], [HB[b]])

        stats(b0)
        recip(b0)
        for b in range(b0, b1):
            if b + 1 < b1:
                stats(b + 1)
            apply(b)
            if b + 1 < b1:
                recip(b + 1)

    P.dma("pool", ident, ident_d[:, :], writes=[b_ident])
    P.dma("sp", vecs, vecs_d[:, :], writes=[b_vecs])
    P.dma("sp", rb18, bass.AP(router_b_h, 0, [[0, 128], [0, 18], [1, 16]]), writes=[b_rb18])
    P.dma("pool", rwb, router_w_d.rearrange("(c p) e -> p c e", p=128), writes=[b_rwb])
    P.op("dve", lambda e: e.memset(ones, 1.0), [], [b_ones])
    P.op("dve", lambda e: e.memset(epsc[:, 0:1], D * EPS), [], [b_eps])
    P.op("dve", lambda e: e.memset(epsc[:, 1:2], EPS), [], [b_eps])
    P.op("act", lambda e: e.activation(out=scb.rearrange("p c t -> p (c t)"), in_=vecs[:, V_CV:V_CV + 16], func=AF.Silu),
         [b_vecs], [b_scb])
    P.op("dve", lambda e: e.tensor_scalar(out=nS, in0=vecs[:, V_NMIX:V_NMIX + 40], scalar1=32.0, scalar2=None, op0=ALU.mult),
         [b_vecs], [b_nS])

    A = Arena()
    wpc = [A.alloc(8192, BF16).rearrange("p (c n) -> p c n", c=8) for _ in range(3)]
    b_wpc = [Buf(f"wpc{i}") for i in range(3)]
    wrr = RR(range(3))
    for layer in range(2):
        pbk = 1 + layer
        for nb in range(12):
            wi = wrr.next()
            load_piece(wpc[wi], b_wpc[wi], w_ada_d[layer, :, nb * 512:(nb + 1) * 512])
            for j in range(4):
                fc = nb * 4 + j
                for kc in range(8):
                    mm(bank(pbk)[:, 2 * fc:2 * fc + 2], wpc[wi][:, kc, j * 128:(j + 1) * 128], scb[:, kc, :],
                       kc == 0, kc == 7, [b_wpc[wi], b_scb], [psb[pbk]], kc == 7 and j == 3)
        pv = bank(pbk)[:, 0:96].rearrange("p (f t) -> p f t", t=2)
        for t in range(2):
            P.op("dve", lambda e, t=t, layer=layer, pv=pv: e.tensor_tensor(
                out=modT[layer][:, t, :], in0=pv[:, :, t], in1=vecs[:, V_BADA + 48 * layer:V_BADA + 48 * (layer + 1)],
                op=ALU.add), [psb[pbk], b_vecs], [b_mod])
        for t in range(2):
            for kind in (0, 2, 3, 5):
                P.op("dve", lambda e, t=t, kind=kind, layer=layer: e.tensor_copy(
                    out=der[layer][:, kind, t, :], in_=modT[layer][:, t, kind * 8:kind * 8 + 8]), [b_mod], [b_der])
            for kind, nofs in ((1, 0), (4, 16)):
                P.op("dve", lambda e, t=t, kind=kind, nofs=nofs, layer=layer: e.scalar_tensor_tensor(
                    out=der[layer][:, kind, t, :], in0=modT[layer][:, t, kind * 8:kind * 8 + 8], scalar=1.0,
                    in1=nS[:, nofs + 8 * layer:nofs + 8 * layer + 8], op0=ALU.add, op1=ALU.mult), [b_mod, b_nS], [b_der])

    def load_transposed(A, src_d, ntiles, dst, dstbufs_of_tile):
        xin = [A.alloc(4096) for _ in range(2)]
        hi = [A.alloc(2048, BF16) for _ in range(2)]
        lo = [A.alloc(2048, BF16) for _ in range(2)]
        o1 = [A.alloc(4096) for _ in range(2)]
        b_xin = [Buf("xin0"), Buf("xin1")]
        b_hi = [Buf("hi0"), Buf("hi1")]
        b_lo = [Buf("lo0"), Buf("lo1")]
        b_o1 = [Buf("o10"), Buf("o11")]
        for t in range(ntiles):
            s = t % 2
            P.dma("sp", xin[s], src_d[t * 128:(t + 1) * 128, :], writes=[b_xin[s]])
            P.op("dve", lambda e, s=s: e.tensor_copy(out=hi[s], in_=xin[s]), [b_xin[s]], [b_hi[s]])
            P.op("dve", lambda e, s=s: e.tensor_tensor(out=lo[s], in0=xin[s], in1=hi[s], op=ALU.subtract),
                 [b_xin[s], b_hi[s]], [b_lo[s]])
            bh, bl = 4 + 2 * s, 5 + 2 * s
            for c in range(8):
                P.op("pe", lambda e, c=c, s=s, bh=bh: e.transpose(bankbf(bh)[:, c * 128:(c + 1) * 128],
                                                                hi[s][:, c * 128:(c + 1) * 128], ident),
                     [b_hi[s], b_ident], [psb[bh]], inc=(c == 7))
            for c in range(8):
                P.op("pe", lambda e, c=c, s=s, bl=bl: e.transpose(bankbf(bl)[:, c * 128:(c + 1) * 128],
                                                                lo[s][:, c * 128:(c + 1) * 128], ident),
                     [b_lo[s], b_ident], [psb[bl]], inc=(c == 7))
            P.op("act", lambda e, s=s, bh=bh: e.activation(out=o1[s], in_=bankbf(bh), func=AF.Copy), [psb[bh]], [b_o1[s]])
            P.op("dve", lambda e, s=s, bl=bl, t=t: e.tensor_tensor(
                out=dst[:, :, t * 128:(t + 1) * 128], in0=o1[s].rearrange("p (c n) -> p c n", c=8),
                in1=bankbf(bl).rearrange("p (c n) -> p c n", c=8), op=ALU.add), [b_o1[s], psb[bl]], [dstbufs_of_tile(t)])

    load_transposed(A, x_d, 16, XT, lambda t: XB[t // 4])
    load_transposed(A, ctx_d, 2, CT, lambda t: XB[4])
    if debug:
        dbg["mod"] = nc.dram_tensor("dbg_mod", [128, 2 * 96], F32, kind="ExternalOutput").ap()
        P.dma("sp", dbg["mod"][:, 0:96], modT[0].rearrange("p t f -> p (t f)"), reads=[b_mod], writes=[Buf("dm0")])
        P.dma("sp", dbg["mod"][:, 96:192], modT[1].rearrange("p t f -> p (t f)"), reads=[b_mod], writes=[Buf("dm1")])

    def dump_any(name, ap, shape, dt, reads):
        if not debug:
            return
        d = nc.dram_tensor("dbg_" + name, list(shape), dt, kind="ExternalOutput").ap()
        P.dma("sp", d, ap, reads=list(reads), writes=[Buf("dd_" + name)])

    def dump_x(tag):
        if not debug:
            return
        d1 = nc.dram_tensor("dbg_x_" + tag, [128, 8 * T], F32, kind="ExternalOutput").ap()
        d2 = nc.dram_tensor("dbg_c_" + tag, [128, 8 * C], F32, kind="ExternalOutput").ap()
        P.dma("sp", d1.rearrange("p (c n) -> p c n", c=8), XT, reads=XB[:4], writes=[Buf("dx" + tag)])
        P.dma("sp", d2.rearrange("p (c n) -> p c n", c=8), CT, reads=[XB[4]], writes=[Buf("dc" + tag)])

    def dump_h(tag):
        if not debug:
            return
        d1 = nc.dram_tensor("dbg_h_" + tag, [128, 8 * TT], BF16, kind="ExternalOutput").ap()
        P.dma("sp", d1.rearrange("p (c n) -> p c n", c=8), HT, reads=HB, writes=[Buf("dh" + tag)])

    def finish():
        P.barrier()
        F = P.eng["sp"]
        P.emit()
        return nc, dbg

    if stop <= 1:
        dump_x("in")
        return finish()

    P.barrier()
    A = Arena()
    norm_mod(A, 0, 5, 1, 0, 0)
    if stop <= 2:
        dump_h("h0")
        return finish()

    P.barrier()
    A = Arena()
    wpc = [A.alloc(8192, BF16).rearrange("p (c n) -> p c n", c=8) for _ in range(3)]
    b_wpc = [Buf(f"wpc{i}") for i in range(3)]
    wrr = RR(range(3))
    rowst = [A.alloc(TT * 2, BF16) for _ in range(2)]
    b_rowst = [Buf("rowst0"), Buf("rowst1")]
    vst = [A.alloc(1024, BF16) for _ in range(4)]
    b_vst = [Buf(f"vst{i}") for i in range(4)]
    prr = RR([1, 2, 3, 4])
    rsr = RR(range(2))
    vsr = RR(range(4))

    def proj_fm(wsrc_cols, dst_s, dst_b, nblk, scale):
        wi = wrr.next()
        load_piece(wpc[wi], b_wpc[wi], wsrc_cols)
        return wi

    for pc in range(4):
        wi = wrr.next()
        load_piece(wpc[wi], b_wpc[wi], na_w_in_d[:, pc * 512:(pc + 1) * 512])
        dst_s, dst_b = (qT_s, qT_b) if pc < 2 else (kT_s, kT_b)
        scale = (32.0 ** -0.5) if pc < 2 else None
        for j in range(4):
            fc = (pc % 2) * 4 + j
            ri = rsr.next()
            for b in range(5):
                t0, n = BLK[b]
                pb = prr.next()
                for kc in range(8):
                    mm(bank(pb)[:, :n], wpc[wi][:, kc, j * 128:(j + 1) * 128], HT[:, kc, t0:t0 + n],
                       kc == 0, kc == 7, [b_wpc[wi], HB[b]], [psb[pb]], kc == 7)
                copy_any(evac_rr.next(), rowst[ri][:, t0:t0 + n], bank(pb)[:, :n], [psb[pb]], [b_rowst[ri]], scale)
            P.dma("sp", dst_s[fc * 128:(fc + 1) * 128, :], rowst[ri], reads=[b_rowst[ri]], writes=[dst_b])
    v33 = v_s[0:33 * 128 * 1024].rearrange("(t p f) -> t p f", p=128, f=1024)
    vtok = [128 * a for a in range(16)] + [64 + 128 * a for a in range(15)] + [2048, 2176]
    for pc in range(4, 6):
        wi = wrr.next()
        load_piece(wpc[wi], b_wpc[wi], na_w_in_d[:, pc * 512:(pc + 1) * 512])
        for ti in range(33):
            tk = vtok[ti]
            pb = prr.next()
            rb_ = [HB[min(tk // 512, 4)], HB[min((tk + 127) // 512, 4)]]
            for kc in range(8):
                mm(bank(pb), HT[:, kc, tk:tk + 128], wpc[wi][:, kc, :], kc == 0, kc == 7,
                   [b_wpc[wi]] + rb_, [psb[pb]], kc == 7)
            vi = vsr.next()
            copy_any(evac_rr.next(), vst[vi], bank(pb), [psb[pb]], [b_vst[vi]])
            P.dma("sp", v33[ti, :, (pc - 4) * 512:(pc - 3) * 512], vst[vi], reads=[b_vst[vi]], writes=[v_b])

    if stop <= 2.5:
        return finish()
    P.barrier()
    A = Arena()
    OT = HT
    OB = HB
    Qc = [A.alloc(TT * 2, BF16) for _ in range(2)]
    Kc = [A.alloc(TT * 2, BF16) for _ in range(2)]
    Vc = [A.alloc(33 * 128 * 2, BF16).rearrange("p (t f) -> p t f", f=128) for _ in range(2)]
    Bt = [A.alloc(4 * 14 * 64 * 2, BF16).rearrange("p (h e q) -> p h e q", h=4, e=14) for _ in range(2)]
    b_Qc, b_Kc, b_Vc, b_Bt = ([Buf(f"{n}{i}") for i in range(2)] for n in ("Qc", "Kc", "Vc", "Bt"))
    PT = [A.alloc(4 * 384 * 2, BF16).rearrange("p (h k) -> p h k", h=4) for _ in range(2)]
    b_PT = [Buf("PT0"), Buf("PT1")]
    rec = A.alloc(2048)
    b_rec = Buf("rec")
    wo = [A.alloc(8192, BF16).rearrange("p (c n) -> p c n", c=8) for _ in range(2)]
    b_wo = [Buf("wo0"), Buf("wo1")]
    for pc in range(2):
        load_piece(wo[pc], b_wo[pc], na_w_out_d[:, pc * 512:(pc + 1) * 512])
    natab_v = natab_d.rearrange("p (h e q) -> p h e q", h=32, e=14)
    ps4 = ps[:, 0:2048].rearrange("p (h k) -> p h k", h=4)

    def units():
        for u in range(36):
            yield u

    for c in range(8):
        s = c % 2
        P.dma("sp", Qc[s], qT_s[c * 128:(c + 1) * 128, :], reads=[qT_b], writes=[b_Qc[s]])
        P.dma("sp", Kc[s], kT_s[c * 128:(c + 1) * 128, :], reads=[kT_b], writes=[b_Kc[s]])
        P.dma("sp", Vc[s], v33[:, :, c * 128:(c + 1) * 128].rearrange("t p f -> p t f"), reads=[v_b], writes=[b_Vc[s]])
        P.dma("pool", Bt[s], natab_v[:, 4 * c:4 * c + 4, :, :], writes=[b_Bt[s]])

        P.op("act", lambda e: e.activation(out=Bt[s], in_=Bt[s], func=AF.Exp), [b_Bt[s]], [b_Bt[s]])

        def qk(u):
            q0 = 64 * u if u < 32 else 2048 + 64 * (u - 32)
            local = u < 32
            if local:
                r = u
                r0 = min(max(r - 4, 0), 24)
                if r0 % 2 == 0:
                    kcol = [128 * (r0 // 2 + i) for i in range(4)]
                else:
                    kcol = [64 + 128 * ((r0 - 1) // 2 + i) for i in range(4)]
            for hl in range(4):
                hs = slice(32 * hl, 32 * hl + 32)
                rd = [b_Qc[s], b_Kc[s]]
                if local:
                    for i in range(4):
                        mm(bank(hl)[:, 64 * i:64 * i + 64], Kc[s][hs, kcol[i]:kcol[i] + 128], Qc[s][hs, q0:q0 + 64],
                           True, True, rd, [psb[hl]], False, tile_position=(32 * hl, 0), skip_group_check=True)
                for t in range(2):
                    mm(bank(hl)[:, 256 + 64 * t:320 + 64 * t], Kc[s][hs, 2048 + 128 * t:2176 + 128 * t],
                       Qc[s][hs, q0:q0 + 64], True, True, rd, [psb[hl]], t == 1,
                       tile_position=(32 * hl, 0), skip_group_check=True)

        def ex(u):
            pi = u % 2
            lo = 0 if u < 32 else 256
            for hh in range(2):
                hp = slice(2 * hh, 2 * hh + 2)
                P.op("act", lambda e: e.activation(out=PT[pi][:, hp, lo:384], in_=ps4[:, hp, lo:384], func=AF.Exp),
                     psb[2 * hh:2 * hh + 2], [b_PT[pi]])
            if u < 32:
                r0 = min(max(u - 4, 0), 24)
                e0 = 7 - (u - r0)
                pl = PT[pi][:, :, 0:256].rearrange("p h (i q) -> p h i q", i=4)
                P.op("dve", lambda e: e.tensor_tensor(out=pl, in0=pl, in1=Bt[s][:, :, e0:e0 + 7:2, :], op=ALU.mult),
                     [b_PT[pi], b_Bt[s]], [b_PT[pi]])

        def pv(u):
            pi = u % 2
            grp = u // 8
            nb_, db_ = (4, 5) if grp % 2 == 0 else (6, 7)
            sl = u % 8
            if u < 32:
                r = u
                r0 = min(max(r - 4, 0), 24)
                if r0 % 2 == 0:
                    vidx = [r0 // 2 + i for i in range(4)]
                else:
                    vidx = [16 + (r0 - 1) // 2 + i for i in range(4)]
                tiles = [(vidx[i], 64 * i) for i in range(4)] + [(31, 256), (32, 320)]
            else:
                tiles = [(31, 256), (32, 320)]
            nt = len(tiles)
            for hl in range(4):
                hs = slice(32 * hl, 32 * hl + 32)
                for k, (vi, pc_) in enumerate(tiles):
                    mm(bank(nb_)[hs, 64 * sl:64 * sl + 64], Vc[s][:, vi, hs], PT[pi][:, hl, pc_:pc_ + 64],
                       k == 0, k == nt - 1, [b_Vc[s], b_PT[pi]], [psb[nb_]], False,
                       tile_position=(0, 32 * hl), skip_group_check=True)
                    mm(bank(db_)[hs, 64 * sl:64 * sl + 64], ones[:, 0:32], PT[pi][:, hl, pc_:pc_ + 64],
                       k == 0, k == nt - 1, [b_ones, b_PT[pi]], [psb[db_]], (hl == 3 and k == nt - 1),
                       tile_position=(0, 32 * hl), skip_group_check=True)

        def fin_q(grp, q):
            nb_, db_ = (4, 5) if grp % 2 == 0 else (6, 7)
            cs = slice(128 * q, 128 * q + 128)
            tok0 = (512 * grp if grp < 4 else 2048) + 128 * q
            P.op("dve", lambda e: e.reciprocal(out=rec[:, cs], in_=bank(db_)[:, cs]), [psb[db_]], [b_rec])
            P.op("dve", lambda e: e.tensor_tensor(out=OT[:, c, tok0:tok0 + 128], in0=bank(nb_)[:, cs],
                                                  in1=rec[:, cs], op=ALU.mult),
                 [psb[nb_], b_rec], [OB[min(grp, 4)]])

        qk(0)
        for u in range(36):
            ex(u)
            if u >= 8 and u % 8 < 4:
                fin_q(u // 8 - 1, u % 8)
            if u + 1 < 36:
                qk(u + 1)
            pv(u)
        fin_q(4, 0)
        fin_q(4, 1)
    if stop <= 3:
        dump_h("o0")
        if debug:
            for nm, src, shp in (("q", qT_s, [D, TT]), ("k", kT_s, [D, TT])):
                dd = nc.dram_tensor("dbg_" + nm, shp, BF16, kind="ExternalOutput").ap()
                P.dma("sp", dd[:, :], src[:, :], reads=[qT_b, kT_b], writes=[Buf("dd" + nm)])
            dd = nc.dram_tensor("dbg_v", [33 * 128, 1024], BF16, kind="ExternalOutput").ap()
            P.dma("sp", dd[:, :], v_s[0:33 * 128 * 1024].rearrange("(t f) -> t f", f=1024), reads=[v_b], writes=[Buf("ddv")])
        return finish()

    def out_proj_residual(wsb, b_wsb, nkc, src, srcB, layer, gkind, blocks, prr_):
        for pc, (wt, wb_) in enumerate(zip(wsb, b_wsb)):
            for j in range(4):
                dc = pc * 4 + j
                for b in blocks:
                    t0, n = BLK[b]
                    t = 0 if b < 4 else 1
                    pb = prr_.next()
                    for kc in range(nkc):
                        mm(bank(pb)[:, :n], wt[:, kc, j * 128:(j + 1) * 128], src[:, kc, t0:t0 + n],
                           kc == 0, kc == nkc - 1, [wb_, srcB[b]], [psb[pb]], kc == nkc - 1)
                    xv = xt_blk(b)
                    P.op("dve", lambda e, pb=pb, n=n, xv=xv, dc=dc, t=t: e.scalar_tensor_tensor(
                        out=xv[:, dc, :], in0=bank(pb)[:, :n], scalar=der[layer][:, gkind, t, dc:dc + 1], in1=xv[:, dc, :],
                        op0=ALU.mult, op1=ALU.add), [psb[pb], b_der, XB[b]], [XB[b]])

    out_proj_residual(wo, b_wo, 8, OT, OB, 0, 2, range(5), RR([0, 1, 2, 3]))
    if stop <= 4:
        dump_x("mix0")
        return finish()

    def moe(layer, nblk):
        P.barrier()
        A = Arena()
        ntile = 18 if nblk == 5 else 16
        norm_mod(A, 0, nblk, 4, 3, layer)
        P.barrier()
        A = Arena()
        lg = A.alloc(18 * 16 * 4).rearrange("p (t e) -> p t e", e=16)
        b_lg = Buf("lg")
        for ti in range(ntile):
            b = min(ti // 4, 4)
            for kc in range(8):
                mm(bank(1)[:, 16 * ti:16 * ti + 16], HT[:, kc, ti * 128:(ti + 1) * 128], rwb[:, kc, :],
                   kc == 0, kc == 7, [HB[b], b_rwb], [psb[1]], (kc == 7 and ti == ntile - 1))
        NE = ntile * 16
        NG = ntile * 4
        sc_ = A.alloc(18 * 16 * 4)
        sel_ = A.alloc(18 * 16 * 4)
        t1_ = A.alloc(18 * 16 * 4)
        t2_ = A.alloc(18 * 16 * 4)
        m1_ = A.alloc(72 * 4)
        m2_ = A.alloc(72 * 4)
        gs_ = A.alloc(72 * 4)
        gm_ = A.alloc(18 * 4)
        ws_ = A.alloc(18 * 4)
        gbf = A.alloc(18 * 16 * 2, BF16)
        gT = A.alloc(TT * 2, BF16)
        selm = A.alloc(16 * 128 * 2, BF16).rearrange("p (e m) -> p e m", e=16)
        b_r = Buf("route")
        b_gbf, b_gT, b_selm = Buf("gbf"), Buf("gT"), Buf("selm")
        P.dma("pool", selm[0:16], sel_d.rearrange("k (e m) -> k e m", e=16), writes=[b_selm])
        g4 = lambda ap: ap[:, :NE].rearrange("p (g k) -> p g k", k=4)
        bc4 = lambda ap: ap[:, :NG].unsqueeze(2).to_broadcast([128, NG, 4])
        P.op("act", lambda e: e.activation(out=sc_[:, :NE], in_=bank(1)[:, :NE], func=AF.Sigmoid), [psb[1]], [b_r])
        P.op("dve", lambda e: e.tensor_tensor(out=sel_[:, :NE], in0=sc_[:, :NE], in1=rb18.rearrange("p t e -> p (t e)")[:, :NE],
                                              op=ALU.add), [b_r, b_rb18], [b_r])
        P.op("dve", lambda e: e.tensor_reduce(out=m1_[:, :NG], in_=g4(sel_), axis=AX.X, op=ALU.max), [b_r], [b_r])
        P.op("dve", lambda e: e.tensor_tensor(out=g4(t1_), in0=g4(sel_), in1=bc4(m1_), op=ALU.is_equal), [b_r], [b_r])
        P.op("dve", lambda e: e.scalar_tensor_tensor(out=t2_[:, :NE], in0=t1_[:, :NE], scalar=-1e9, in1=sel_[:, :NE],
                                                     op0=ALU.mult, op1=ALU.add), [b_r], [b_r])
        P.op("dve", lambda e: e.tensor_reduce(out=m2_[:, :NG], in_=g4(t2_), axis=AX.X, op=ALU.max), [b_r], [b_r])
        P.op("dve", lambda e: e.tensor_tensor(out=gs_[:, :NG], in0=m1_[:, :NG], in1=m2_[:, :NG], op=ALU.add), [b_r], [b_r])
        P.op("dve", lambda e: e.tensor_reduce(out=gm_[:, :ntile], in_=gs_[:, :NG].rearrange("p (t g) -> p t g", g=4),
                                              axis=AX.X, op=ALU.max), [b_r], [b_r])
        P.op("dve", lambda e: e.tensor_tensor(out=m1_[:, :NG].rearrange("p (t g) -> p t g", g=4),
                                              in0=gs_[:, :NG].rearrange("p (t g) -> p t g", g=4),
                                              in1=gm_[:, :ntile].unsqueeze(2).to_broadcast([128, ntile, 4]), op=ALU.is_equal),
             [b_r], [b_r])
        P.op("dve", lambda e: e.tensor_tensor(out=g4(t1_), in0=g4(sel_), in1=bc4(m2_), op=ALU.is_ge), [b_r], [b_r])
        P.op("dve", lambda e: e.tensor_tensor(out=g4(t1_), in0=g4(t1_), in1=bc4(m1_), op=ALU.mult), [b_r], [b_r])
        P.op("dve", lambda e: e.tensor_tensor(out=t2_[:, :NE], in0=t1_[:, :NE], in1=sc_[:, :NE], op=ALU.mult), [b_r], [b_r])
        P.op("dve", lambda e: e.tensor_reduce(out=ws_[:, :ntile], in_=t2_[:, :NE].rearrange("p (t e) -> p t e", e=16),
                                              axis=AX.X, op=ALU.add), [b_r], [b_r])
        P.op("dve", lambda e: e.reciprocal(out=ws_[:, :ntile], in_=ws_[:, :ntile]), [b_r], [b_r])
        P.op("dve", lambda e: e.tensor_tensor(out=gbf[:, :NE].rearrange("p (t e) -> p t e", e=16),
                                              in0=t2_[:, :NE].rearrange("p (t e) -> p t e", e=16),
                                              in1=ws_[:, :ntile].unsqueeze(2).to_broadcast([128, ntile, 16]), op=ALU.mult),
             [b_r], [b_gbf])
        for ti in range(ntile):
            pb = 2 + ti // 8
            o = (ti % 8) * 128
            P.op("pe", lambda e, ti=ti, pb=pb, o=o: e.transpose(bankbf(pb)[0:16, o:o + 128], gbf[:, 16 * ti:16 * ti + 16], ident),
                 [b_gbf, b_ident], [psb[pb]], inc=(ti % 8 == 7 or ti == ntile - 1))
        for g in range((ntile + 7) // 8):
            nn = min(8, ntile - 8 * g) * 128
            P.op("dve", lambda e, g=g, nn=nn: e.tensor_copy(out=gT[0:16, 1024 * g:1024 * g + nn], in_=bankbf(2 + g)[0:16, :nn]),
                 [psb[2 + g]], [b_gT])
        wE = [[A.alloc(8192, BF16) for _ in range(3)] for _ in range(2)]
        b_wE = [[Buf(f"wE{i}{j}") for j in range(3)] for i in range(2)]
        hid = [A.alloc(4 * 512 * 2, BF16).rearrange("p (f n) -> p f n", f=4) for _ in range(2)]
        b_hid = [Buf("hid0"), Buf("hid1")]
        sg = [A.alloc(1024, BF16) for _ in range(2)]
        b_sg = [Buf("sg0"), Buf("sg1")]
        gbc = [A.alloc(1024, BF16) for _ in range(2)]
        b_gbc = [Buf("gbc0"), Buf("gbc1")]
        k_g = RR([1, 2])
        k_u = RR([3, 4])
        k_d = RR([5, 6])
        wts = {}

        def gate_up(ex_, b, hb):
            s = ex_ % 2
            wg_t, wu_t, _ = wts[ex_]
            t0, n = BLK[b]
            mm(bank(7)[:, :n], selm[0:16, ex_, :], gT[0:16, t0:t0 + n], True, True, [b_selm, b_gT], [psb[7]], True)
            P.op("act", lambda e: e.activation(out=gbc[hb][:, :n], in_=bank(7)[:, :n], func=AF.Copy),
                 [psb[7]], [b_gbc[hb]])
            for f in range(4):
                pg, pu = k_g.next(), k_u.next()
                for kc in range(8):
                    mm(bank(pg)[:, :n], wg_t[:, kc, f * 128:(f + 1) * 128], HT[:, kc, t0:t0 + n],
                       kc == 0, kc == 7, [b_wE[s][0], HB[b]], [psb[pg]], kc == 7)
                for kc in range(8):
                    mm(bank(pu)[:, :n], wu_t[:, kc, f * 128:(f + 1) * 128], HT[:, kc, t0:t0 + n],
                       kc == 0, kc == 7, [b_wE[s][1], HB[b]], [psb[pu]], kc == 7)
                si = f % 2
                P.op("act", lambda e: e.activation(out=sg[si][:, :n], in_=bank(pg)[:, :n], func=AF.Silu),
                     [psb[pg]], [b_sg[si]])
                P.op("dve", lambda e: e.tensor_tensor(out=sg[si][:, :n], in0=sg[si][:, :n],
                                                      in1=gbc[hb][:, :n], op=ALU.mult),
                     [b_sg[si], b_gbc[hb]], [b_sg[si]])
                P.op("dve", lambda e: e.tensor_tensor(
                    out=hid[hb][:, f, :n], in0=bank(pu)[:, :n], in1=sg[si][:, :n], op=ALU.mult),
                    [psb[pu], b_sg[si]], [b_hid[hb]])

        def down(ex_, b, hb):
            s = ex_ % 2
            wd_t = wts[ex_][2]
            t0, n = BLK[b]
            t = 0 if b < 4 else 1
            xv = xt_blk(b)
            for dc in range(8):
                pd = k_d.next()
                for f in range(4):
                    mm(bank(pd)[:, :n], wd_t[:, f, dc * 128:(dc + 1) * 128], hid[hb][:, f, :n],
                       f == 0, f == 3, [b_wE[s][2], b_hid[hb]], [psb[pd]], f == 3)
                P.op("dve", lambda e: e.scalar_tensor_tensor(
                    out=xv[:, dc, :], in0=bank(pd)[:, :n], scalar=der[layer][:, 5, t, dc:dc + 1], in1=xv[:, dc, :],
                    op0=ALU.mult, op1=ALU.add), [psb[pd], b_der, XB[b]], [XB[b]])

        prev = None
        cnt = 0
        for ex_ in range(16):
            s = ex_ % 2
            wg_t = wE[s][0].rearrange("p (c n) -> p c n", c=8)
            wu_t = wE[s][1].rearrange("p (c n) -> p c n", c=8)
            wd_t = wE[s][2].rearrange("p (c n) -> p c n", c=4)
            wts[ex_] = (wg_t, wu_t, wd_t)
            P.dma("pool", wg_t, wg_d[layer, ex_].rearrange("(c p) n -> p c n", p=128), writes=[b_wE[s][0]])
            P.dma("pool", wu_t, wu_d[layer, ex_].rearrange("(c p) n -> p c n", p=128), writes=[b_wE[s][1]])
            P.dma("pool", wd_t, wd_d[layer, ex_].rearrange("(c p) n -> p c n", p=128), writes=[b_wE[s][2]])
            for b in range(nblk):
                hb = cnt % 2
                cnt += 1
                gate_up(ex_, b, hb)
                if prev is not None:
                    down(*prev)
                prev = (ex_, b, hb)
        down(*prev)

    moe(0, 5)
    if stop <= 5:
        dump_x("out0")
        return finish()

    P.barrier()
    A = Arena()
    norm_mod(A, 0, 5, 1, 0, 1)
    if stop <= 6:
        dump_h("h1")
        return finish()
    P.barrier()
    A = Arena()
    wpc = [A.alloc(8192, BF16).rearrange("p (c n) -> p c n", c=8) for _ in range(3)]
    b_wpc = [Buf(f"wpc{i}") for i in range(3)]
    wrr = RR(range(3))
    cosT = A.alloc(T * 4)
    sinT = A.alloc(T * 4)
    b_cs = Buf("cossin")
    P.dma("sp", cosT, cos_d[:, :], writes=[b_cs])
    P.dma("sp", sinT, sin_d[:, :], writes=[b_cs])
    ra = [A.alloc(2048) for _ in range(2)]
    rb_ = [A.alloc(2048) for _ in range(2)]
    b_ra = [Buf("ra0"), Buf("ra1")]
    b_rbb = [Buf("rb0"), Buf("rb1")]
    rt2 = [[A.alloc(2048) for _ in range(2)] for _ in range(4)]
    b_rt2 = [[Buf(f"rt{i}_{j}") for j in range(2)] for i in range(4)]
    rows1 = [A.alloc(TT * 2, BF16) for _ in range(2)]
    rows2 = [A.alloc(TT * 2, BF16) for _ in range(2)]
    b_rows1 = [Buf("rows10"), Buf("rows11")]
    b_rows2 = [Buf("rows20"), Buf("rows21")]
    vst = [A.alloc(1024, BF16) for _ in range(4)]
    b_vst = [Buf(f"vst{i}") for i in range(4)]
    vsr = RR(range(4))
    pA = RR([1, 3])
    pB = RR([2, 4])
    prr = RR([5, 6, 7])
    hcount = 0
    for pc in range(4):
        wi = wrr.next()
        load_piece(wpc[wi], b_wpc[wi], ret_w_in_d[:, pc * 512:(pc + 1) * 512])
        isq = pc < 2
        dst_s, dst_b = (qT_s, qT_b) if isq else (kT_s, kT_b)
        for hh in range(2):
            hd = (pc % 2) * 2 + hh
            ri = hcount % 2
            hcount += 1
            nb_ = 4 if isq else 5
            for b in range(nb_):
                t0, n = BLK[b]
                p1, p2 = pA.next(), pB.next()
                for half, pb in ((0, p1), (1, p2)):
                    j = hh * 2 + half
                    for kc in range(8):
                        mm(bank(pb)[:, :n], wpc[wi][:, kc, j * 128:(j + 1) * 128], HT[:, kc, t0:t0 + n],
                           kc == 0, kc == 7, [b_wpc[wi], HB[b]], [psb[pb]], kc == 7)
                if b < 4:
                    ai = b % 2
                    P.op("act", lambda e, ai=ai, p1=p1: e.activation(out=ra[ai], in_=bank(p1), func=AF.Copy), [psb[p1]], [b_ra[ai]])
                    P.op("act", lambda e, ai=ai, p2=p2: e.activation(out=rb_[ai], in_=bank(p2), func=AF.Copy), [psb[p2]], [b_rbb[ai]])
                    cs, sn = cosT[:, t0:t0 + 512], sinT[:, t0:t0 + 512]
                    P.op("dve", lambda e, ai=ai, cs=cs: e.tensor_tensor(out=rt2[0][ai], in0=ra[ai], in1=cs, op=ALU.mult), [b_ra[ai], b_cs], [b_rt2[0][ai]])
                    P.op("dve", lambda e, ai=ai, sn=sn: e.tensor_tensor(out=rt2[1][ai], in0=rb_[ai], in1=sn, op=ALU.mult), [b_rbb[ai], b_cs], [b_rt2[1][ai]])
                    P.op("dve", lambda e, ri=ri, t0=t0: e.tensor_tensor(out=rows1[ri][:, t0:t0 + 512], in0=rt2[0][ai], in1=rt2[1][ai], op=ALU.subtract),
                         [b_rt2[0][ai], b_rt2[1][ai]], [b_rows1[ri]])
                    P.op("pool", lambda e, ai=ai, sn=sn: e.tensor_tensor(out=rt2[2][ai], in0=ra[ai], in1=sn, op=ALU.mult), [b_ra[ai], b_cs], [b_rt2[2][ai]])
                    P.op("pool", lambda e, ai=ai, cs=cs: e.tensor_tensor(out=rt2[3][ai], in0=rb_[ai], in1=cs, op=ALU.mult), [b_rbb[ai], b_cs], [b_rt2[3][ai]])
                    P.op("dve", lambda e, ri=ri, t0=t0: e.tensor_tensor(out=rows2[ri][:, t0:t0 + 512], in0=rt2[2][ai], in1=rt2[3][ai], op=ALU.add),
                         [b_rt2[2][ai], b_rt2[3][ai]], [b_rows2[ri]])
                else:
                    P.op("act", lambda e, ri=ri, p1=p1, t0=t0, n=n: e.activation(out=rows1[ri][:, t0:t0 + n], in_=bank(p1)[:, :n], func=AF.Copy),
                         [psb[p1]], [b_rows1[ri]])
                    P.op("act", lambda e, ri=ri, p2=p2, t0=t0, n=n: e.activation(out=rows2[ri][:, t0:t0 + n], in_=bank(p2)[:, :n], func=AF.Copy),
                         [psb[p2]], [b_rows2[ri]])
            ncol = T if isq else TT
            P.dma("sp", dst_s[(2 * hd) * 128:(2 * hd + 1) * 128, 0:ncol], rows1[ri][:, 0:ncol], reads=[b_rows1[ri]], writes=[dst_b])
            P.dma("sp", dst_s[(2 * hd + 1) * 128:(2 * hd + 2) * 128, 0:ncol], rows2[ri][:, 0:ncol], reads=[b_rows2[ri]], writes=[dst_b])
    v2 = v_s.rearrange("(t f) -> t f", f=2 * D)
    for pc in range(4, 12):
        wi = wrr.next()
        load_piece(wpc[wi], b_wpc[wi], ret_w_in_d[:, pc * 512:(pc + 1) * 512])
        isv = pc < 8
        hd = (pc - 4) % 4
        for ti in range(18 if isv else 16):
            pb = prr.next()
            b = min(ti // 4, 4)
            for kc in range(8):
                mm(bank(pb), HT[:, kc, ti * 128:(ti + 1) * 128], wpc[wi][:, kc, :], kc == 0, kc == 7,
                   [b_wpc[wi], HB[b]], [psb[pb]], kc == 7)
            vi = vsr.next()
            if isv:
                copy_any(evac_rr.next(), vst[vi], bank(pb), [psb[pb]], [b_vst[vi]])
                P.dma("sp", v2[ti * 128:(ti + 1) * 128, hd * 512:(hd + 1) * 512], vst[vi], reads=[b_vst[vi]], writes=[v_b])
            else:
                P.op("act", lambda e, vi=vi, pb=pb: e.activation(out=vst[vi], in_=bank(pb), func=AF.Silu), [psb[pb]], [b_vst[vi]])
                P.dma("sp", sg_s[ti * 128:(ti + 1) * 128, hd * 512:(hd + 1) * 512], vst[vi], reads=[b_vst[vi]], writes=[sg_b])

    if stop <= 6.5:
        return finish()
    P.barrier()
    A = Arena()
    rc = A.alloc(772 * 4)
    b_rc = Buf("rc")
    P.dma("sp", rc, rcst_d[:, :], writes=[b_rc])
    lgt = A.alloc(32)
    b_lg = Buf("lgt")
    P.dma("sp", lgt, bass.AP(ret_dec_h, 0, [[0, 128], [1, 8]]), writes=[b_lg])
    P.op("act", lambda e: e.activation(out=lgt, in_=lgt, func=AF.Exp, scale=-1.0), [b_lg], [b_lg])
    P.op("act", lambda e: e.activation(out=lgt, in_=lgt, func=AF.Ln, bias=1.0), [b_lg], [b_lg])
    P.op("dve", lambda e: e.tensor_scalar(out=lgt, in0=lgt, scalar1=-1.0, scalar2=None, op0=ALU.mult), [b_lg], [b_lg])
    dec = A.alloc(4 * 128 * 4).rearrange("p (h n) -> p h n", h=4)
    rowF = A.alloc(4 * 128 * 4).rearrange("p (h n) -> p h n", h=4)
    rowB = A.alloc(4 * 128 * 4).rearrange("p (h n) -> p h n", h=4)
    colv = A.alloc(4 * 4 * 4).rearrange("p (h k) -> p h k", h=4)
    e1 = A.alloc(512)
    e2 = A.alloc(512)
    b_dec, b_e = Buf("dec"), Buf("e12")
    for hd in range(4):
        lf, lb = lgt[:, hd:hd + 1], lgt[:, 4 + hd:5 + hd]
        P.op("act", lambda e, lf=lf: e.activation(out=e1, in_=rc[:, 0:128], func=AF.Exp, scale=lf), [b_rc, b_lg], [b_e])
        P.op("act", lambda e, lb=lb: e.activation(out=e2, in_=rc[:, 128:256], func=AF.Exp, scale=lb), [b_rc, b_lg], [b_e])
        P.op("dve", lambda e: e.tensor_tensor(out=e1, in0=e1, in1=rc[:, 256:384], op=ALU.mult), [b_e, b_rc], [b_e])
        P.op("dve", lambda e: e.tensor_tensor(out=e2, in0=e2, in1=rc[:, 384:512], op=ALU.mult), [b_e, b_rc], [b_e])
        P.op("dve", lambda e, hd=hd: e.tensor_tensor(out=dec[:, hd, :], in0=e1, in1=e2, op=ALU.add), [b_e], [b_dec])
        P.op("act", lambda e, hd=hd, lf=lf: e.activation(out=rowF[:, hd, :], in_=rc[:, 512:640], func=AF.Exp, scale=lf), [b_rc, b_lg], [b_dec])
        P.op("act", lambda e, hd=hd, lb=lb: e.activation(out=rowB[:, hd, :], in_=rc[:, 640:768], func=AF.Exp, scale=lb), [b_rc, b_lg], [b_dec])
        P.op("act", lambda e, hd=hd, lf=lf: e.activation(out=colv[:, hd, 0:1], in_=rc[:, 768:769], func=AF.Exp, scale=lf), [b_rc, b_lg], [b_dec])
        P.op("act", lambda e, hd=hd, lb=lb: e.activation(out=colv[:, hd, 1:2], in_=rc[:, 769:770], func=AF.Exp, scale=lb), [b_rc, b_lg], [b_dec])
        P.op("act", lambda e, hd=hd, lf=lf: e.activation(out=colv[:, hd, 2:3], in_=rc[:, 770:771], func=AF.Exp, scale=lf), [b_rc, b_lg], [b_dec])
        P.op("act", lambda e, hd=hd, lb=lb: e.activation(out=colv[:, hd, 3:4], in_=rc[:, 770:771], func=AF.Exp, scale=lb), [b_rc, b_lg], [b_dec])
        P.op("dve", lambda e, hd=hd: e.tensor_scalar(out=colv[:, hd, 0:2], in0=colv[:, hd, 0:2], scalar1=1.0 / 16.0, scalar2=None, op0=ALU.mult),
             [b_dec], [b_dec])

    Qh = A.alloc(2 * T * 2, BF16).rearrange("p (c n) -> p c n", c=2)
    QF = A.alloc(2 * T * 2, BF16).rearrange("p (c n) -> p c n", c=2)
    QB = A.alloc(2 * T * 2, BF16).rearrange("p (c n) -> p c n", c=2)
    Kh = A.alloc(2 * TT * 2, BF16).rearrange("p (c n) -> p c n", c=2)
    zT = A.alloc(4 * T * 2, BF16).rearrange("p (c n) -> p c n", c=4)
    woh = A.alloc(4 * 1024 * 2, BF16).rearrange("p (c n) -> p c n", c=4)
    gnh = A.alloc(2048)
    b_Qh, b_QF, b_Kh, b_woh, b_gn = Buf("Qh"), Buf("QF"), Buf("Kh"), Buf("woh"), Buf("gn")
    b_zT = [Buf(f"zT{i}") for i in range(4)]
    b_QFc = [Buf("QFc0"), Buf("QFc1")]
    Vt = [A.alloc(1024, BF16) for _ in range(3)]
    b_Vt = [Buf(f"Vt{i}") for i in range(3)]
    SGt = [A.alloc(1024, BF16) for _ in range(2)]
    b_SGt = [Buf("SGt0"), Buf("SGt1")]
    Sst = [A.alloc(2 * 512 * 4).rearrange("p (c n) -> p c n", c=2) for _ in range(2)]
    b_Sst = [Buf("SstF"), Buf("SstB")]
    Sfb = [A.alloc(2 * 512 * 2, BF16).rearrange("p (c n) -> p c n", c=2) for _ in range(2)]
    b_Sfb = [Buf("Sfb0"), Buf("Sfb1")]
    Ksc = [A.alloc(512, BF16) for _ in range(2)]
    b_Ksc = [Buf("Ksc0"), Buf("Ksc1")]
    Ap = [A.alloc(256, BF16) for _ in range(2)]
    b_Ap = [Buf("Ap0"), Buf("Ap1")]
    z1 = [view(HT_raw_off + 32768 + 2048 * i, 2048) for i in range(2)]
    b_z1 = [Buf("z10"), Buf("z11")]
    zb = [A.alloc(1024, BF16) for _ in range(2)]
    b_zb = [Buf("zb0"), Buf("zb1")]
    junk = A.alloc(1024, BF16)
    b_junk = Buf("junk")
    ssq = A.alloc(32)
    b_ssq = Buf("ssq")
    SbS = view(HT_raw_off, 16 * 2 * 512 * 2, BF16).rearrange("p (c h n) -> p c h n", c=16, h=2)
    b_SbS = [Buf(f"SbS{i}") for i in range(16)]
    vrr = RR(range(3))
    oprr = RR([7, 3, 6])

    def kv_part1(hd, tt, direction, ki):
        vi = vrr.next()
        P.dma("sp", Vt[vi], v2[tt * 128:(tt + 1) * 128, hd * 512:(hd + 1) * 512], reads=[v_b], writes=[b_Vt[vi]])
        c0 = tt * 128
        for half in range(2):
            P.op("pe", lambda e: e.transpose(bankbf(0)[:, half * 128:(half + 1) * 128], Kh[:, half, c0:c0 + 128], ident),
                 [b_Kh, b_ident], [psb[0]], inc=(half == 1))
        copy_any("act" if direction == 0 else "dve", Ksc[ki], bankbf(0)[:, 0:256], [psb[0], b_dec], [b_Ksc[ki]],
                 scale=colv[:, hd, direction:direction + 1])
        return vi

    def kv_part2(hd, direction, first, ki, vi, banks, store_bf=None, store_buf=None):
        for half in range(2):
            pb = banks[half]
            mm(bank(pb), Ksc[ki][:, half * 128:(half + 1) * 128], Vt[vi], True, True, [b_Ksc[ki], b_Vt[vi]], [psb[pb]], True)
            if first:
                P.op("dve", lambda e: e.tensor_copy(out=Sst[direction][:, half, :], in_=bank(pb)),
                     [psb[pb]], [b_Sst[direction]])
            else:
                P.op("dve", lambda e: e.scalar_tensor_tensor(
                    out=Sst[direction][:, half, :], in0=Sst[direction][:, half, :], scalar=colv[:, hd, 2 + direction:3 + direction],
                    in1=bank(pb), op0=ALU.mult, op1=ALU.add), [psb[pb], b_dec, b_Sst[direction]], [b_Sst[direction]])
        if store_bf is not None:
            P.op("act", lambda e: e.activation(out=store_bf, in_=Sst[direction], func=AF.Copy), [b_Sst[direction]], [store_buf])

    def ztrans(c, si):
        c0 = c * 128
        for j in range(4):
            P.op("pe", lambda e: e.transpose(bankbf(6)[:, j * 128:(j + 1) * 128], zb[si][:, j * 128:(j + 1) * 128], ident),
                 [b_zb[si], b_ident], [psb[6]], inc=(j == 3))
        P.op("act", lambda e: e.activation(out=zT[:, :, c0:c0 + 128],
                                           in_=bankbf(6)[:, 0:512].rearrange("p (j n) -> p j n", j=4), func=AF.Copy),
             [psb[6]], [b_zT[c // 4]])

    pending_op = []
    for hd in range(4):
        P.dma("sp", Qh, qT_s[(2 * hd) * 128:(2 * hd + 2) * 128, 0:T].rearrange("(c p) n -> p c n", p=128), reads=[qT_b], writes=[b_Qh])
        P.dma("sp", Kh, kT_s[(2 * hd) * 128:(2 * hd + 2) * 128, :].rearrange("(c p) n -> p c n", p=128), reads=[kT_b], writes=[b_Kh])
        P.dma("sp", gnh, bass.AP(ret_norm_h, hd * 512, [[0, 128], [1, 512]]), writes=[b_gn])
        bw = [(17, True, None), (16, False, 15)] + [(c, False, c - 1) for c in range(15, 0, -1)]
        pend = None
        for i, (tt, first, st) in enumerate(bw):
            ki = i % 2
            vi = kv_part1(hd, tt, 1, ki)
            if pend is not None:
                kv_part2(*pend)
            banks = (1, 2) if i % 2 == 0 else (4, 5)
            pend = (hd, 1, first, ki, vi, banks) + ((SbS[:, st, :, :], b_SbS[st]) if st is not None else (None, None))
            for _ in range(2):
                if pending_op:
                    pending_op.pop(0)()
        kv_part2(*pend)
        while pending_op:
            pending_op.pop(0)()
        P.dma("pool", woh, ret_w_out_d[hd * 512:(hd + 1) * 512, :].rearrange("(c p) n -> p c n", p=128), writes=[b_woh])
        vi = kv_part1(hd, 16, 0, 0)
        kv_part2(hd, 0, True, 0, vi, (1, 2))
        vi = kv_part1(hd, 17, 0, 0)
        kv_part2(hd, 0, False, 0, vi, (1, 2), Sfb[0], b_Sfb[0])
        prevz = None
        for c in range(16):
            si = c % 2
            c0 = c * 128
            if c < 15:
                vi = kv_part1(hd, c, 0, 0)
            else:
                vi = vrr.next()
                P.dma("sp", Vt[vi], v2[c * 128:(c + 1) * 128, hd * 512:(hd + 1) * 512], reads=[v_b], writes=[b_Vt[vi]])
            P.dma("sp", SGt[si], sg_s[c * 128:(c + 1) * 128, hd * 512:(hd + 1) * 512], reads=[sg_b], writes=[b_SGt[si]])
            P.op("dve", lambda e: e.tensor_tensor(out=QF[:, :, c0:c0 + 128], in0=Qh[:, :, c0:c0 + 128],
                                                  in1=rowF[:, hd, :].unsqueeze(1).to_broadcast([128, 2, 128]), op=ALU.mult),
                 [b_Qh, b_dec], [b_QFc[si]])
            P.op("dve", lambda e: e.tensor_tensor(out=QB[:, :, c0:c0 + 128], in0=Qh[:, :, c0:c0 + 128],
                                                  in1=rowB[:, hd, :].unsqueeze(1).to_broadcast([128, 2, 128]), op=ALU.mult),
                 [b_Qh, b_dec], [b_QFc[si]])
            for half in range(2):
                mm(bank(3)[:, 0:128], Kh[:, half, c0:c0 + 128], Qh[:, half, c0:c0 + 128], half == 0, half == 1,
                   [b_Kh, b_Qh], [psb[3]], half == 1)
            P.op("dve", lambda e: e.tensor_tensor(out=Ap[si], in0=bank(3)[:, 0:128], in1=dec[:, hd, :], op=ALU.mult),
                 [psb[3], b_dec], [b_Ap[si]])
            if c < 15:
                kv_part2(hd, 0, False, 0, vi, (1, 2), Sfb[1 - si], b_Sfb[1 - si])
            po = 4 + si
            mm(bank(po), Ap[si], Vt[vi], True, False, [b_Ap[si], b_Vt[vi]], [psb[po]], False)
            for half in range(2):
                mm(bank(po), QF[:, half, c0:c0 + 128], Sfb[si][:, half, :], False, False, [b_QFc[si], b_Sfb[si]], [psb[po]], False)
            for half in range(2):
                mm(bank(po), QB[:, half, c0:c0 + 128], SbS[:, c, half, :], False, half == 1, [b_QFc[si], b_SbS[c]], [psb[po]], half == 1)
            P.op("dve", lambda e: e.memset(ssq[:, 0:1], 0.0), [], [b_ssq])
            P.op("act", lambda e: e.activation(out=junk, in_=bank(po), func=AF.Square, accum_out=ssq[:, 0:1]),
                 [psb[po]], [b_junk, b_ssq])
            P.op("act", lambda e: e.activation(out=ssq[:, 1:2], in_=ssq[:, 0:1], func=AF.Sqrt, scale=1.0 / 512.0, bias=epsc[:, 1:2]),
                 [b_ssq, b_eps], [b_ssq])
            P.op("dve", lambda e: e.reciprocal(out=ssq[:, 2:3], in_=ssq[:, 1:2]), [b_ssq], [b_ssq])
            P.op("dve", lambda e: e.scalar_tensor_tensor(out=z1[si], in0=bank(po), scalar=ssq[:, 2:3], in1=gnh,
                                                         op0=ALU.mult, op1=ALU.mult),
                 [psb[po], b_ssq, b_gn], [b_z1[si]])
            P.op("pool", lambda e: e.tensor_tensor(out=zb[si], in0=z1[si], in1=SGt[si], op=ALU.mult),
                 [b_z1[si], b_SGt[si]], [b_zb[si]])
            if prevz is not None:
                ztrans(*prevz)
            prevz = (c, si)
        ztrans(*prevz)
        def op_group(dc, b):
            t0, n = BLK[b]
            pb = oprr.next()
            for j in range(4):
                mm(bank(pb), woh[:, j, dc * 128:(dc + 1) * 128], zT[:, j, t0:t0 + 512], j == 0, j == 3,
                   [b_woh, b_zT[b]], [psb[pb]], j == 3)
            P.op("dve", lambda e: e.scalar_tensor_tensor(
                out=XT[:, dc, t0:t0 + 512], in0=bank(pb), scalar=der[1][:, 2, 0, dc:dc + 1], in1=XT[:, dc, t0:t0 + 512],
                op0=ALU.mult, op1=ALU.add), [psb[pb], b_der, XB[b]], [XB[b]])
        pending_op = [(lambda dc=dc, b=b: op_group(dc, b)) for dc in range(8) for b in range(4)]
    while pending_op:
        pending_op.pop(0)()
    if stop <= 7:
        dump_x("mix1")
        return finish()

    moe(1, 4)
    if stop <= 8:
        dump_x("out1")
        return finish()

    P.barrier()
    A = Arena()
    sq = [A.alloc(8 * 512 * 2, BF16).rearrange("p (c n) -> p c n", c=8) for _ in range(2)]
    rst = [A.alloc(2048) for _ in range(2)]
    yt = [A.alloc(2048) for _ in range(4)]
    hiT = [A.alloc(8 * 512 * 2, BF16).rearrange("p (c n) -> p c n", c=8) for _ in range(2)]
    loT = [A.alloc(8 * 512 * 2, BF16).rearrange("p (c n) -> p c n", c=8) for _ in range(2)]
    o1 = [A.alloc(4096) for _ in range(2)]
    o2 = [A.alloc(4096) for _ in range(2)]
    b_sq = [[Buf(f"fsq{i}_{c}") for c in range(8)] for i in range(2)]
    b_rst = [Buf("frst0"), Buf("frst1")]
    b_hi = [Buf("fhi0"), Buf("fhi1")]
    b_lo = [Buf("flo0"), Buf("flo1")]
    b_yt = [Buf(f"fyt{i}") for i in range(4)]
    b_o1 = [Buf("fo10"), Buf("fo11")]
    b_o2 = [Buf("fo20"), Buf("fo21")]
    kk = [0, 0]

    def f_stats(b):
        t0 = b * 512
        i = b % 2
        for c in range(8):
            P.op("act", lambda e: e.activation(out=sq[i][:, c, :], in_=XT[:, c, t0:t0 + 512], func=AF.Square), [XB[b]], [b_sq[i][c]])
            mm(bank(0), ones, sq[i][:, c, :], c == 0, c == 7, [b_ones, b_sq[i][c]], [psb[0]], c == 7)
        P.op("act", lambda e: e.activation(out=rst[i], in_=bank(0), func=AF.Sqrt, bias=epsc[:, 0:1]), [psb[0], b_eps], [b_rst[i]])

    def f_recip(b):
        i = b % 2
        P.op("dve", lambda e: e.reciprocal(out=rst[i], in_=rst[i]), [b_rst[i]], [b_rst[i]])

    def f_split(b):
        t0 = b * 512
        i = b % 2
        for c in range(8):
            yi = kk[0] % 4
            kk[0] += 1
            P.op("dve", lambda e: e.scalar_tensor_tensor(
                out=yt[yi], in0=XT[:, c, t0:t0 + 512], scalar=nS[:, 32 + c:33 + c], in1=rst[i], op0=ALU.mult, op1=ALU.mult),
                [XB[b], b_nS, b_rst[i]], [b_yt[yi]])
            P.op("act", lambda e: e.activation(out=hiT[i][:, c, :], in_=yt[yi], func=AF.Copy), [b_yt[yi]], [b_hi[i]])
            P.op("pool", lambda e: e.tensor_tensor(out=loT[i][:, c, :], in0=yt[yi], in1=hiT[i][:, c, :], op=ALU.subtract),
                 [b_yt[yi], b_hi[i]], [b_lo[i]])

    def f_emit(b):
        t0 = b * 512
        i = b % 2
        for s4 in range(4):
            oi = kk[1] % 2
            kk[1] += 1
            bh, bl = 1 + 2 * oi, 2 + 2 * oi
            for c in range(8):
                P.op("pe", lambda e: e.transpose(bankbf(bh)[:, c * 128:(c + 1) * 128],
                                                 hiT[i][:, c, s4 * 128:(s4 + 1) * 128], ident),
                     [b_hi[i], b_ident], [psb[bh]], inc=(c == 7))
            for c in range(8):
                P.op("pe", lambda e: e.transpose(bankbf(bl)[:, c * 128:(c + 1) * 128],
                                                 loT[i][:, c, s4 * 128:(s4 + 1) * 128], ident),
                     [b_lo[i], b_ident], [psb[bl]], inc=(c == 7))
            P.op("act", lambda e: e.activation(out=o1[oi], in_=bankbf(bh), func=AF.Copy), [psb[bh]], [b_o1[oi]])
            P.op("dve", lambda e: e.tensor_tensor(out=o2[oi], in0=o1[oi], in1=bankbf(bl), op=ALU.add),
                 [b_o1[oi], psb[bl]], [b_o2[oi]])
            r0_ = t0 + s4 * 128
            P.dma("sp", out_d[r0_:r0_ + 128, :], o2[oi], reads=[b_o2[oi]], writes=[out_b])

    f_stats(0)
    f_recip(0)
    for b in range(4):
        if b + 1 < 4:
            f_stats(b + 1)
        f_split(b)
        if b + 1 < 4:
            f_recip(b + 1)
        f_emit(b)
    return finish()


_CACHE = {}


def _prep_inputs(inp):
    f32 = np.float32
    g = lambda k: np.asarray(inp[k], dtype=f32)
    x, c, ctx, c_ctx = g("x"), g("c"), g("ctx"), g("c_ctx")
    w_ada, b_ada = g("w_ada"), g("b_ada")
    shared = {
        "w_ada": np.ascontiguousarray(w_ada),
        "na_w_in": np.ascontiguousarray(g("na_w_in")[0]),
        "na_w_out": np.ascontiguousarray(g("na_w_out")[0]),
        "natab": _na_table(g("na_rpb")[0]),
        "ret_w_in": np.ascontiguousarray(g("ret_w_in")[0]),
        "ret_w_out": np.ascontiguousarray(g("ret_w_out")[0]),
        "ret_dec": np.concatenate([g("ret_decay_fwd")[0], g("ret_decay_bwd")[0]])[None, :].astype(f32),
        "ret_norm": np.ascontiguousarray(g("ret_norm")),
        "rcst": _ret_consts(),
        "router_w": np.ascontiguousarray(g("router_w")),
        "router_b": g("router_b")[None, :].copy(),
        "moe_w_gate": np.ascontiguousarray(g("moe_w_gate")),
        "moe_w_up": np.ascontiguousarray(g("moe_w_up")),
        "moe_w_down": np.ascontiguousarray(g("moe_w_down")),
        "ident": np.eye(128, dtype=f32),
        "sel": np.ascontiguousarray(np.repeat(np.eye(16, dtype=f32)[:, :, None], 128, axis=2).reshape(16, 16 * 128)),
    }
    cs, sn = _rot_tables()
    shared["rcos"], shared["rsin"] = cs, sn
    maps = []
    for b in range(8):
        vecs = np.zeros((128, NV), f32)
        cv = np.stack([_fm(c[b]), _fm(c_ctx)], axis=-1)
        vecs[:, V_CV:V_CV + 16] = cv.reshape(128, 16)
        for i in range(2):
            vecs[:, V_BADA + 48 * i:V_BADA + 48 * (i + 1)] = _fm(b_ada[i])
            vecs[:, V_NMIX + 8 * i:V_NMIX + 8 * (i + 1)] = _fm(g("norm_mix")[i])
            vecs[:, V_NFFN + 8 * i:V_NFFN + 8 * (i + 1)] = _fm(g("norm_ffn")[i])
        vecs[:, V_FN:V_FN + 8] = _fm(g("final_norm"))
        m = dict(shared)
        m["x"] = np.ascontiguousarray(x[b])
        m["ctx"] = np.ascontiguousarray(ctx[b])
        m["vecs"] = vecs
        maps.append(m)
    return maps


def kernel(**inputs):
    if "nc" not in _CACHE:
        _CACHE["nc"] = build()[0]
    nc = _CACHE["nc"]
    maps = _prep_inputs(inputs)
    res = run_bass_kernel_spmd(nc, maps, core_ids=list(range(8)))
    return np.stack([np.asarray(r["out"], dtype=np.float32) for r in res.results], axis=0)
```

```python
import numpy as np
import concourse.bass as bass
import concourse.mybir as mybir
from concourse.bass_utils import run_bass_kernel_spmd

F32 = mybir.dt.float32
BF16 = mybir.dt.bfloat16
AF = mybir.ActivationFunctionType
ALU = mybir.AluOpType
AX = mybir.AxisListType

D = 1024
T = 2048
C = 256
TT = T + C
EPS = 1e-6
SEM_CH = 16000


class Buf:
    __slots__ = ("name", "w", "rs", "dsem", "dcount")

    def __init__(self, name):
        self.name = name
        self.w = None
        self.rs = []
        self.dsem = None
        self.dcount = 0


class Eng:
    def __init__(self, name):
        self.name = name
        self.count = 0
        self.sems = []
        self.seen = {}
        self.seen_d = {}
        self.ops = []


class _Rec:
    def __init__(self):
        self.call = None

    def __getattr__(self, name):
        def f(*a, **k):
            assert self.call is None
            self.call = (name, a, k)
            return None
        return f


class Prog:
    ENGS = ("pe", "act", "dve", "pool", "sp")

    def __init__(self, nc):
        self.nc = nc
        self.eng = {n: Eng(n) for n in self.ENGS}
        self.nsem = 0
        self.dbufs = []

    def new_sem(self, name):
        self.nsem += 1
        return self.nc.semaphore(f"s{self.nsem}_{name}").__enter__()

    def _sem_for(self, E, m):
        k = (m - 1) // SEM_CH
        while len(E.sems) <= k:
            E.sems.append(self.new_sem(f"{E.name}{len(E.sems)}"))
        return E.sems[k], (m - 1) % SEM_CH + 1

    def _need(self, F, tok, waits):
        if tok is None:
            return
        if tok[0] == "E":
            _, en, m = tok
            if F.seen.get(en, 0) >= m:
                return
            F.seen[en] = m
            waits.append(self._sem_for(self.eng[en], m))
        else:
            _, b, c = tok
            if F.seen_d.get(id(b), 0) >= c:
                return
            F.seen_d[id(b)] = c
            waits.append((b.dsem, c))

    def _deps(self, F, reads, writes, is_dma=False):
        waits = []
        for b in reads:
            t = b.w
            if t is None:
                continue
            if t[0] == "E" and t[1] == F.name and F.name == "pe" and not is_dma:
                continue
            self._need(F, t, waits)
        for b in writes:
            for t in [b.w] + b.rs:
                if t is None:
                    continue
                if t[0] == "E" and t[1] == F.name and F.name == "pe" and not is_dma:
                    continue
                self._need(F, t, waits)
        return waits

    def op(self, en, fn, reads=(), writes=(), inc=True, lowp=None):
        rec = _Rec()
        fn(rec)
        name_, a_, k_ = rec.call
        nc_ = self.nc

        def fn(e, name_=name_, a_=a_, k_=k_):
            if lowp is not None:
                with nc_.allow_low_precision(reason=lowp):
                    return getattr(e, name_)(*a_, **k_)
            return getattr(e, name_)(*a_, **k_)
        F = self.eng[en]
        waits = self._deps(F, reads, writes)
        m = F.count + 1
        tok = ("E", en, m)
        if inc:
            F.count = m
            sem, _ = self._sem_for(F, m)
            F.ops.append((waits, fn, (sem, 1)))
        else:
            F.ops.append((waits, fn, None))
        for b in reads:
            b.rs.append(tok)
        for b in writes:
            b.w = tok
            b.rs = []
        return tok

    def dma(self, q, out, in_, reads=(), writes=(), **kw):
        F = self.eng[q]
        waits = self._deps(F, reads, writes, is_dma=True)
        wb = writes[0]
        if wb.dsem is None:
            wb.dsem = self.new_sem("d_" + wb.name)
            self.dbufs.append(wb)
        wb.dcount += 16
        tok = ("D", wb, wb.dcount)
        sem = wb.dsem

        def fn(e, out=out, in_=in_, kw=kw):
            return e.dma_start(out=out, in_=in_, **kw)
        F.ops.append((waits, fn, (sem, 16)))
        for b in reads:
            b.rs.append(tok)
        for b in writes:
            b.w = tok
            b.rs = []
        return tok

    def barrier(self):
        for F in self.eng.values():
            waits = []
            for E in self.eng.values():
                if E is not F and E.count > 0:
                    self._need(F, ("E", E.name, E.count), waits)
            for b in self.dbufs:
                if b.dcount:
                    self._need(F, ("D", b, b.dcount), waits)
            F.ops.append((waits, None, None))

    def emit(self):
        nc = self.nc

        def replay(e, ops):
            for waits, fn, inc in ops:
                for sem, val in waits:
                    e.wait_ge(sem, val)
                if fn is None:
                    continue
                ins = fn(e)
                if inc is not None:
                    ins.then_inc(inc[0], inc[1])

        with nc.Block() as block:
            @block.tensor
            def _(e):
                replay(e, self.eng["pe"].ops)

            @block.scalar
            def _(e):
                replay(e, self.eng["act"].ops)

            @block.vector
            def _(e):
                replay(e, self.eng["dve"].ops)

            @block.gpsimd
            def _(e):
                replay(e, self.eng["pool"].ops)

            @block.sync
            def _(e):
                replay(e, self.eng["sp"].ops)


class RR:
    def __init__(self, items):
        self.items = list(items)
        self.i = 0

    def next(self):
        v = self.items[self.i % len(self.items)]
        self.i += 1
        return v


NV = 152
V_CV, V_BADA, V_NMIX, V_NFFN, V_FN = 0, 16, 112, 128, 144


def _fm(v):
    return np.ascontiguousarray(v.reshape(-1, 128).T)


def _na_table(rpb):
    H = rpb.shape[0]
    kc = np.arange(64)[:, None]
    qc = np.arange(64)[None, :]
    qstart = np.clip(qc - 8, 0, 48)
    valid = (kc >= qstart) & (kc < qstart + 16)
    dx = np.clip(kc - qc + 15, 0, 30)
    tab = np.empty((2, 64, H, 14, 64), np.float32)
    for j2 in range(2):
        for e in range(14):
            g = rpb[:, e + j2, :][:, dx]
            g = np.where(valid[None], g, np.float32(-1e30))
            tab[j2, :, :, e, :] = g.transpose(1, 0, 2)
    return tab.reshape(128, H * 14 * 64)


def _ret_consts():
    j = np.arange(128, dtype=np.float32)[:, None]
    i = np.arange(128, dtype=np.float32)[None, :]
    r = np.zeros((128, 128 * 6 + 4), np.float32)
    r[:, 0:128] = np.maximum(i - j, 0)
    r[:, 128:256] = np.maximum(j - i, 0)
    r[:, 256:384] = (i >= j) / 16.0
    r[:, 384:512] = (j >= i) / 16.0
    r[:, 512:640] = i + 1.0
    r[:, 640:768] = 128.0 - i
    r[:, 768] = 127.0 - j[:, 0]
    r[:, 769] = j[:, 0]
    r[:, 770] = 128.0
    r[:, 771] = 1.0 / 16.0
    return r


def _rot_tables():
    theta = (1.0 / (10000.0 ** np.linspace(0.0, 1.0, 128, dtype=np.float32))).astype(np.float32)
    n = np.arange(T, dtype=np.float32)
    ang = (n[None, :] * theta[:, None]).astype(np.float32)
    return np.cos(ang.astype(np.float64)).astype(np.float32), np.sin(ang.astype(np.float64)).astype(np.float32)


def build(stop=99, debug=False):
    nc = bass.Bass("TRN2", target_bir_lowering=False)
    P = Prog(nc)

    def din(name, shape):
        return nc.dram_tensor(name, list(shape), F32, kind="ExternalInput")

    x_d = din("x", [T, D]).ap()
    ctx_d = din("ctx", [C, D]).ap()
    vecs_d = din("vecs", [128, NV]).ap()
    w_ada_d = din("w_ada", [2, D, 6 * D]).ap()
    na_w_in_d = din("na_w_in", [D, 3 * D]).ap()
    na_w_out_d = din("na_w_out", [D, D]).ap()
    natab_d = din("natab", [128, 32 * 14 * 64]).ap()
    ret_w_in_d = din("ret_w_in", [D, 6 * D]).ap()
    ret_w_out_d = din("ret_w_out", [2 * D, D]).ap()
    ret_dec_h = din("ret_dec", [1, 8])
    ret_norm_h = din("ret_norm", [1, 2 * D])
    rcst_d = din("rcst", [128, 772]).ap()
    cos_d = din("rcos", [128, T]).ap()
    sin_d = din("rsin", [128, T]).ap()
    router_w_d = din("router_w", [D, 16]).ap()
    router_b_h = din("router_b", [1, 16])
    wg_d = din("moe_w_gate", [2, 16, D, 512]).ap()
    wu_d = din("moe_w_up", [2, 16, D, 512]).ap()
    wd_d = din("moe_w_down", [2, 16, 512, D]).ap()
    ident_d = din("ident", [128, 128]).ap()
    sel_d = din("sel", [16, 16 * 128]).ap()
    out_d = nc.dram_tensor("out", [T, D], F32, kind="ExternalOutput").ap()
    qT_s = nc.dram_tensor("qT_s", [D, TT], BF16).ap()
    kT_s = nc.dram_tensor("kT_s", [D, TT], BF16).ap()
    v_s = nc.dram_tensor("v_s", [TT * 2 * D], BF16).ap()
    sg_s = nc.dram_tensor("sg_s", [T, 2 * D], BF16).ap()
    qT_b, kT_b, v_b, sg_b, out_b = Buf("qT_s"), Buf("kT_s"), Buf("v_s"), Buf("sg_s"), Buf("out")
    dbg = {}

    AB = 208000
    arena = nc.alloc_sbuf_tensor("arena", [128, AB // 4], F32)
    ps = nc.alloc_psum_tensor("ps", [128, 4096], F32)
    psb = [Buf(f"ps{i}") for i in range(8)]

    def bank(i):
        return ps[:, 512 * i:512 * (i + 1)]

    def bankbf(i):
        return ps[:, 512 * i:512 * (i + 1)].bitcast(BF16)

    def view(off, nbytes, dt=F32):
        assert off % 4 == 0 and nbytes % 4 == 0 and off + nbytes <= AB, (off, nbytes)
        a_ = arena[:, off // 4:(off + nbytes) // 4]
        return a_ if dt == F32 else a_.bitcast(dt)

    XT = view(0, 65536).rearrange("p (c n) -> p c n", c=8)
    CT = view(65536, 8192).rearrange("p (c n) -> p c n", c=8)
    HT = view(73728, 36864, BF16).rearrange("p (c n) -> p c n", c=8)
    HT_raw_off = 73728
    pp = [110592]

    def palloc(nbytes, dt=F32):
        o = pp[0]
        pp[0] += (nbytes + 31) // 32 * 32
        return view(o, nbytes, dt)

    ident = palloc(256, BF16)
    ones = palloc(256, BF16)
    vecs = palloc(NV * 4)
    modT = [palloc(2 * 48 * 4).rearrange("p (t f) -> p t f", t=2) for _ in range(2)]
    der = [palloc(6 * 2 * 8 * 4).rearrange("p (k t c) -> p k t c", k=6, t=2) for _ in range(2)]
    nS = palloc(40 * 4)
    epsc = palloc(32)
    scb = palloc(8 * 2 * 2, BF16).rearrange("p (c t) -> p c t", t=2)
    rwb = palloc(8 * 16 * 2, BF16).rearrange("p (c e) -> p c e", e=16)
    rb18 = palloc(18 * 16 * 4).rearrange("p (t e) -> p t e", e=16)
    ARENA0 = (pp[0] + 63) // 64 * 64
    assert ARENA0 <= 119808, ARENA0
    b_ident, b_ones, b_vecs, b_mod, b_der, b_nS, b_eps, b_scb, b_rwb, b_rb18 = (Buf(n) for n in
        ("ident", "ones", "vecs", "mod", "der", "nS", "eps", "scb", "rwb", "rb18"))
    XB = [Buf(f"xt{i}") for i in range(4)] + [Buf("ct")]
    HB = [Buf(f"ht{i}") for i in range(5)]
    BLK = [(i * 512, 512) for i in range(4)] + [(2048, 256)]

    def xt_blk(b):
        if b < 4:
            return XT[:, :, b * 512:(b + 1) * 512]
        return CT[:, :, :]

    class Arena:
        def __init__(self):
            self.o = ARENA0

        def alloc(self, nbytes, dt=F32):
            o = self.o
            self.o += (nbytes + 63) // 64 * 64
            assert self.o <= AB, ("arena overflow", self.o)
            return view(o, nbytes, dt)

    def mm(out, lhsT, rhs, start, stop, reads, writes, inc, **kw):
        P.op("pe", lambda e: e.matmul(out, lhsT=lhsT, rhs=rhs, start=start, stop=stop, **kw), reads, writes, inc=inc)

    evac_rr = RR(["act", "dve"])

    def copy_any(eng, out, in_, reads, writes, scale=None):
        if eng == "act":
            if scale is None:
                P.op("act", lambda e: e.activation(out=out, in_=in_, func=AF.Copy), reads, writes)
            else:
                P.op("act", lambda e: e.activation(out=out, in_=in_, func=AF.Identity, scale=scale), reads, writes)
        else:
            if scale is None:
                P.op(eng, lambda e: e.tensor_copy(out=out, in_=in_), reads, writes)
            else:
                P.op(eng, lambda e: e.tensor_scalar(out=out, in0=in_, scalar1=scale, scalar2=None, op0=ALU.mult), reads, writes)

    def load_piece(dst, dstb, src_rows_cols):
        P.dma("pool", dst, src_rows_cols.rearrange("(c p) n -> p c n", p=128), writes=[dstb])

    def norm_mod(A, b0, b1, kA, kS, layer, dst_is_final=False):
        sq = [A.alloc(8 * 512 * 2, BF16).rearrange("p (c n) -> p c n", c=8) for _ in range(2)]
        rst = [A.alloc(512 * 4) for _ in range(2)]
        NT = 8
        tmp = [A.alloc(512 * 4) for _ in range(NT)]
        b_sq = [[Buf(f"sq{i}_{c}") for c in range(8)] for i in range(2)]
        b_rst = [Buf("rst0"), Buf("rst1")]
        b_tmp = [Buf(f"nt{i}") for i in range(NT)]
        kk = [0]

        def stats(b):
            t0, n = BLK[b]
            i = b % 2
            src = xt_blk(b)
            for c in range(8):
                P.op("act", lambda e: e.activation(out=sq[i][:, c, :n], in_=src[:, c, :], func=AF.Square),
                     [XB[b]], [b_sq[i][c]])
                mm(bank(0)[:, :n], ones, sq[i][:, c, :n], c == 0, c == 7, [b_ones, b_sq[i][c]], [psb[0]], c == 7)

        def recip(b):
            t0, n = BLK[b]
            i = b % 2
            P.op("act", lambda e: e.activation(out=rst[i][:, :n], in_=bank(0)[:, :n], func=AF.Sqrt, bias=epsc[:, 0:1]),
                 [psb[0], b_eps], [b_rst[i]])
            P.op("dve", lambda e: e.reciprocal(out=rst[i][:, :n], in_=rst[i][:, :n]), [b_rst[i]], [b_rst[i]])

        def apply(b):
            t0, n = BLK[b]
            t = 0 if b < 4 else 1
            i = b % 2
            src = xt_blk(b)
            for c in range(8):
                tb = kk[0] % NT
                kk[0] += 1
                P.op("dve", lambda e: e.scalar_tensor_tensor(
                    out=tmp[tb][:, :n], in0=src[:, c, :], scalar=der[layer][:, kA, t, c:c + 1], in1=rst[i][:, :n],
                    op0=ALU.mult, op1=ALU.mult), [XB[b], b_der, b_rst[i]], [b_tmp[tb]])
                P.op("act", lambda e: e.activation(
                    out=HT[:, c, t0:t0 + n], in_=tmp[tb][:, :n], func=AF.Identity, bias=der[layer][:, kS, t, c:c + 1]),
                    [b_tmp[tb], b_der], [HB[b]])

        stats(b0)
        recip(b0)
        for b in range(b0, b1):
            if b + 1 < b1:
                stats(b + 1)
            apply(b)
            if b + 1 < b1:
                recip(b + 1)

    P.dma("pool", ident, ident_d[:, :], writes=[b_ident])
    P.dma("sp", vecs, vecs_d[:, :], writes=[b_vecs])
    P.dma("sp", rb18, bass.AP(router_b_h, 0, [[0, 128], [0, 18], [1, 16]]), writes=[b_rb18])
    P.dma("pool", rwb, router_w_d.rearrange("(c p) e -> p c e", p=128), writes=[b_rwb])
    P.op("dve", lambda e: e.memset(ones, 1.0), [], [b_ones])
    P.op("dve", lambda e: e.memset(epsc[:, 0:1], D * EPS), [], [b_eps])
    P.op("dve", lambda e: e.memset(epsc[:, 1:2], EPS), [], [b_eps])
    P.op("act", lambda e: e.activation(out=scb.rearrange("p c t -> p (c t)"), in_=vecs[:, V_CV:V_CV + 16], func=AF.Silu),
         [b_vecs], [b_scb])
    P.op("dve", lambda e: e.tensor_scalar(out=nS, in0=vecs[:, V_NMIX:V_NMIX + 40], scalar1=32.0, scalar2=None, op0=ALU.mult),
         [b_vecs], [b_nS])

    A = Arena()
    wpc = [A.alloc(8192, BF16).rearrange("p (c n) -> p c n", c=8) for _ in range(3)]
    b_wpc = [Buf(f"wpc{i}") for i in range(3)]
    wrr = RR(range(3))
    for layer in range(2):
        pbk = 1 + layer
        for nb in range(12):
            wi = wrr.next()
            load_piece(wpc[wi], b_wpc[wi], w_ada_d[layer, :, nb * 512:(nb + 1) * 512])
            for j in range(4):
                fc = nb * 4 + j
                for kc in range(8):
                    mm(bank(pbk)[:, 2 * fc:2 * fc + 2], wpc[wi][:, kc, j * 128:(j + 1) * 128], scb[:, kc, :],
                       kc == 0, kc == 7, [b_wpc[wi], b_scb], [psb[pbk]], kc == 7 and j == 3)
        pv = bank(pbk)[:, 0:96].rearrange("p (f t) -> p f t", t=2)
        for t in range(2):
            P.op("dve", lambda e, t=t, layer=layer, pv=pv: e.tensor_tensor(
                out=modT[layer][:, t, :], in0=pv[:, :, t], in1=vecs[:, V_BADA + 48 * layer:V_BADA + 48 * (layer + 1)],
                op=ALU.add), [psb[pbk], b_vecs], [b_mod])
        for t in range(2):
            for kind in (0, 2, 3, 5):
                P.op("dve", lambda e, t=t, kind=kind, layer=layer: e.tensor_copy(
                    out=der[layer][:, kind, t, :], in_=modT[layer][:, t, kind * 8:kind * 8 + 8]), [b_mod], [b_der])
            for kind, nofs in ((1, 0), (4, 16)):
                P.op("dve", lambda e, t=t, kind=kind, nofs=nofs, layer=layer: e.scalar_tensor_tensor(
                    out=der[layer][:, kind, t, :], in0=modT[layer][:, t, kind * 8:kind * 8 + 8], scalar=1.0,
                    in1=nS[:, nofs + 8 * layer:nofs + 8 * layer + 8], op0=ALU.add, op1=ALU.mult), [b_mod, b_nS], [b_der])

    def load_transposed(A, src_d, ntiles, dst, dstbufs_of_tile):
        xin = [A.alloc(4096) for _ in range(2)]
        hi = [A.alloc(2048, BF16) for _ in range(2)]
        lo = [A.alloc(2048, BF16) for _ in range(2)]
        o1 = [A.alloc(4096) for _ in range(2)]
        b_xin = [Buf("xin0"), Buf("xin1")]
        b_hi = [Buf("hi0"), Buf("hi1")]
        b_lo = [Buf("lo0"), Buf("lo1")]
        b_o1 = [Buf("o10"), Buf("o11")]
        for t in range(ntiles):
            s = t % 2
            P.dma("sp", xin[s], src_d[t * 128:(t + 1) * 128, :], writes=[b_xin[s]])
            P.op("dve", lambda e, s=s: e.tensor_copy(out=hi[s], in_=xin[s]), [b_xin[s]], [b_hi[s]])
            P.op("dve", lambda e, s=s: e.tensor_tensor(out=lo[s], in0=xin[s], in1=hi[s], op=ALU.subtract),
                 [b_xin[s], b_hi[s]], [b_lo[s]])
            bh, bl = 4 + 2 * s, 5 + 2 * s
            for c in range(8):
                P.op("pe", lambda e, c=c, s=s, bh=bh: e.transpose(bankbf(bh)[:, c * 128:(c + 1) * 128],
                                                                hi[s][:, c * 128:(c + 1) * 128], ident),
                     [b_hi[s], b_ident], [psb[bh]], inc=(c == 7))
            for c in range(8):
                P.op("pe", lambda e, c=c, s=s, bl=bl: e.transpose(bankbf(bl)[:, c * 128:(c + 1) * 128],
                                                                lo[s][:, c * 128:(c + 1) * 128], ident),
                     [b_lo[s], b_ident], [psb[bl]], inc=(c == 7))
            P.op("act", lambda e, s=s, bh=bh: e.activation(out=o1[s], in_=bankbf(bh), func=AF.Copy), [psb[bh]], [b_o1[s]])
            P.op("dve", lambda e, s=s, bl=bl, t=t: e.tensor_tensor(
                out=dst[:, :, t * 128:(t + 1) * 128], in0=o1[s].rearrange("p (c n) -> p c n", c=8),
                in1=bankbf(bl).rearrange("p (c n) -> p c n", c=8), op=ALU.add), [b_o1[s], psb[bl]], [dstbufs_of_tile(t)])

    load_transposed(A, x_d, 16, XT, lambda t: XB[t // 4])
    load_transposed(A, ctx_d, 2, CT, lambda t: XB[4])
    if debug:
        dbg["mod"] = nc.dram_tensor("dbg_mod", [128, 2 * 96], F32, kind="ExternalOutput").ap()
        P.dma("sp", dbg["mod"][:, 0:96], modT[0].rearrange("p t f -> p (t f)"), reads=[b_mod], writes=[Buf("dm0")])
        P.dma("sp", dbg["mod"][:, 96:192], modT[1].rearrange("p t f -> p (t f)"), reads=[b_mod], writes=[Buf("dm1")])

    def dump_any(name, ap, shape, dt, reads):
        if not debug:
            return
        d = nc.dram_tensor("dbg_" + name, list(shape), dt, kind="ExternalOutput").ap()
        P.dma("sp", d, ap, reads=list(reads), writes=[Buf("dd_" + name)])

    def dump_x(tag):
        if not debug:
            return
        d1 = nc.dram_tensor("dbg_x_" + tag, [128, 8 * T], F32, kind="ExternalOutput").ap()
        d2 = nc.dram_tensor("dbg_c_" + tag, [128, 8 * C], F32, kind="ExternalOutput").ap()
        P.dma("sp", d1.rearrange("p (c n) -> p c n", c=8), XT, reads=XB[:4], writes=[Buf("dx" + tag)])
        P.dma("sp", d2.rearrange("p (c n) -> p c n", c=8), CT, reads=[XB[4]], writes=[Buf("dc" + tag)])

    def dump_h(tag):
        if not debug:
            return
        d1 = nc.dram_tensor("dbg_h_" + tag, [128, 8 * TT], BF16, kind="ExternalOutput").ap()
        P.dma("sp", d1.rearrange("p (c n) -> p c n", c=8), HT, reads=HB, writes=[Buf("dh" + tag)])

    def finish():
        P.barrier()
        F = P.eng["sp"]
        P.emit()
        return nc, dbg

    if stop <= 1:
        dump_x("in")
        return finish()

    P.barrier()
    A = Arena()
    norm_mod(A, 0, 5, 1, 0, 0)
    if stop <= 2:
        dump_h("h0")
        return finish()

    P.barrier()
    A = Arena()
    wpc = [A.alloc(8192, BF16).rearrange("p (c n) -> p c n", c=8) for _ in range(3)]
    b_wpc = [Buf(f"wpc{i}") for i in range(3)]
    wrr = RR(range(3))
    rowst = [A.alloc(TT * 2, BF16) for _ in range(2)]
    b_rowst = [Buf("rowst0"), Buf("rowst1")]
    vst = [A.alloc(1024, BF16) for _ in range(4)]
    b_vst = [Buf(f"vst{i}") for i in range(4)]
    prr = RR([1, 2, 3, 4])
    rsr = RR(range(2))
    vsr = RR(range(4))

    def proj_fm(wsrc_cols, dst_s, dst_b, nblk, scale):
        wi = wrr.next()
        load_piece(wpc[wi], b_wpc[wi], wsrc_cols)
        return wi

    for pc in range(4):
        wi = wrr.next()
        load_piece(wpc[wi], b_wpc[wi], na_w_in_d[:, pc * 512:(pc + 1) * 512])
        dst_s, dst_b = (qT_s, qT_b) if pc < 2 else (kT_s, kT_b)
        scale = (32.0 ** -0.5) if pc < 2 else None
        for j in range(4):
            fc = (pc % 2) * 4 + j
            ri = rsr.next()
            for b in range(5):
                t0, n = BLK[b]
                pb = prr.next()
                for kc in range(8):
                    mm(bank(pb)[:, :n], wpc[wi][:, kc, j * 128:(j + 1) * 128], HT[:, kc, t0:t0 + n],
                       kc == 0, kc == 7, [b_wpc[wi], HB[b]], [psb[pb]], kc == 7)
                copy_any(evac_rr.next(), rowst[ri][:, t0:t0 + n], bank(pb)[:, :n], [psb[pb]], [b_rowst[ri]], scale)
            P.dma("sp", dst_s[fc * 128:(fc + 1) * 128, :], rowst[ri], reads=[b_rowst[ri]], writes=[dst_b])
    vtm = v_s[0:TT * 1024].rearrange("(t f) -> t f", f=1024)
    vtok = [128 * a for a in range(18)]
    for pc in range(4, 6):
        wi = wrr.next()
        load_piece(wpc[wi], b_wpc[wi], na_w_in_d[:, pc * 512:(pc + 1) * 512])
        for ti in range(18):
            tk = vtok[ti]
            pb = prr.next()
            rb_ = [HB[min(tk // 512, 4)], HB[min((tk + 127) // 512, 4)]]
            for kc in range(8):
                mm(bank(pb), HT[:, kc, tk:tk + 128], wpc[wi][:, kc, :], kc == 0, kc == 7,
                   [b_wpc[wi]] + rb_, [psb[pb]], kc == 7)
            vi = vsr.next()
            copy_any(evac_rr.next(), vst[vi], bank(pb), [psb[pb]], [b_vst[vi]])
            P.dma("sp", vtm[tk:tk + 128, (pc - 4) * 512:(pc - 3) * 512], vst[vi], reads=[b_vst[vi]], writes=[v_b])

    if stop <= 2.5:
        return finish()
    P.barrier()
    A = Arena()
    OT = HT
    OB = HB
    Qc = [A.alloc(TT * 2, BF16) for _ in range(2)]
    Kc = [A.alloc(TT * 2, BF16) for _ in range(2)]
    Vc = [A.alloc(33 * 128 * 2, BF16).rearrange("p (t f) -> p t f", f=128) for _ in range(2)]
    Bt = [A.alloc(4 * 14 * 64 * 2, BF16).rearrange("p (h e q) -> p h e q", h=4, e=14) for _ in range(2)]
    b_Qc, b_Kc, b_Vc, b_Bt = ([Buf(f"{n}{i}") for i in range(2)] for n in ("Qc", "Kc", "Vc", "Bt"))
    PT = [A.alloc(4 * 384 * 2, BF16).rearrange("p (h k) -> p h k", h=4) for _ in range(2)]
    b_PT = [Buf("PT0"), Buf("PT1")]
    rec = A.alloc(2048)
    b_rec = Buf("rec")
    wo = [A.alloc(8192, BF16).rearrange("p (c n) -> p c n", c=8) for _ in range(2)]
    b_wo = [Buf("wo0"), Buf("wo1")]
    for pc in range(2):
        load_piece(wo[pc], b_wo[pc], na_w_out_d[:, pc * 512:(pc + 1) * 512])
    natab_v = natab_d.rearrange("p (h e q) -> p h e q", h=32, e=14)
    ps4 = ps[:, 0:2048].rearrange("p (h k) -> p h k", h=4)

    def units():
        for u in range(36):
            yield u

    for c in range(8):
        s = c % 2
        P.dma("sp", Qc[s], qT_s[c * 128:(c + 1) * 128, :], reads=[qT_b], writes=[b_Qc[s]])
        P.dma("sp", Kc[s], kT_s[c * 128:(c + 1) * 128, :], reads=[kT_b], writes=[b_Kc[s]])
        cs_ = slice(c * 128, (c + 1) * 128)
        P.dma("sp", Vc[s][:, 0:16, :], vtm[0:2048, cs_].rearrange("(t p) f -> p t f", p=128), reads=[v_b], writes=[b_Vc[s]])
        P.dma("sp", Vc[s][:, 16:31, :], vtm[64:1984, cs_].rearrange("(t p) f -> p t f", p=128), reads=[v_b], writes=[b_Vc[s]])
        P.dma("sp", Vc[s][:, 31:33, :], vtm[2048:2304, cs_].rearrange("(t p) f -> p t f", p=128), reads=[v_b], writes=[b_Vc[s]])
        P.dma("pool", Bt[s], natab_v[:, 4 * c:4 * c + 4, :, :], writes=[b_Bt[s]])

        P.op("act", lambda e: e.activation(out=Bt[s], in_=Bt[s], func=AF.Exp), [b_Bt[s]], [b_Bt[s]])

        def qk(u):
            q0 = 64 * u if u < 32 else 2048 + 64 * (u - 32)
            local = u < 32
            if local:
                r = u
                r0 = min(max(r - 4, 0), 24)
                if r0 % 2 == 0:
                    kcol = [128 * (r0 // 2 + i) for i in range(4)]
                else:
                    kcol = [64 + 128 * ((r0 - 1) // 2 + i) for i in range(4)]
            for hl in range(4):
                hs = slice(32 * hl, 32 * hl + 32)
                rd = [b_Qc[s], b_Kc[s]]
                if local:
                    for i in range(4):
                        mm(bank(hl)[:, 64 * i:64 * i + 64], Kc[s][hs, kcol[i]:kcol[i] + 128], Qc[s][hs, q0:q0 + 64],
                           True, True, rd, [psb[hl]], False, tile_position=(32 * hl, 0), skip_group_check=True)
                for t in range(2):
                    mm(bank(hl)[:, 256 + 64 * t:320 + 64 * t], Kc[s][hs, 2048 + 128 * t:2176 + 128 * t],
                       Qc[s][hs, q0:q0 + 64], True, True, rd, [psb[hl]], t == 1,
                       tile_position=(32 * hl, 0), skip_group_check=True)

        def ex(u):
            pi = u % 2
            lo = 0 if u < 32 else 256
            for hh in range(2):
                hp = slice(2 * hh, 2 * hh + 2)
                P.op("act", lambda e: e.activation(out=PT[pi][:, hp, lo:384], in_=ps4[:, hp, lo:384], func=AF.Exp),
                     psb[2 * hh:2 * hh + 2], [b_PT[pi]])
            if u < 32:
                r0 = min(max(u - 4, 0), 24)
                e0 = 7 - (u - r0)
                pl = PT[pi][:, :, 0:256].rearrange("p h (i q) -> p h i q", i=4)
                P.op("dve", lambda e: e.tensor_tensor(out=pl, in0=pl, in1=Bt[s][:, :, e0:e0 + 7:2, :], op=ALU.mult),
                     [b_PT[pi], b_Bt[s]], [b_PT[pi]])

        def pv(u):
            pi = u % 2
            grp = u // 8
            nb_, db_ = (4, 5) if grp % 2 == 0 else (6, 7)
            sl = u % 8
            if u < 32:
                r = u
                r0 = min(max(r - 4, 0), 24)
                if r0 % 2 == 0:
                    vidx = [r0 // 2 + i for i in range(4)]
                else:
                    vidx = [16 + (r0 - 1) // 2 + i for i in range(4)]
                tiles = [(vidx[i], 64 * i) for i in range(4)] + [(31, 256), (32, 320)]
            else:
                tiles = [(31, 256), (32, 320)]
            nt = len(tiles)
            for hl in range(4):
                hs = slice(32 * hl, 32 * hl + 32)
                for k, (vi, pc_) in enumerate(tiles):
                    mm(bank(nb_)[hs, 64 * sl:64 * sl + 64], Vc[s][:, vi, hs], PT[pi][:, hl, pc_:pc_ + 64],
                       k == 0, k == nt - 1, [b_Vc[s], b_PT[pi]], [psb[nb_]], False,
                       tile_position=(0, 32 * hl), skip_group_check=True)
                    mm(bank(db_)[hs, 64 * sl:64 * sl + 64], ones[:, 0:32], PT[pi][:, hl, pc_:pc_ + 64],
                       k == 0, k == nt - 1, [b_ones, b_PT[pi]], [psb[db_]], (hl == 3 and k == nt - 1),
                       tile_position=(0, 32 * hl), skip_group_check=True)

        def fin_q(grp, q):
            nb_, db_ = (4, 5) if grp % 2 == 0 else (6, 7)
            cs = slice(128 * q, 128 * q + 128)
            tok0 = (512 * grp if grp < 4 else 2048) + 128 * q
            P.op("dve", lambda e: e.reciprocal(out=rec[:, cs], in_=bank(db_)[:, cs]), [psb[db_]], [b_rec])
            P.op("dve", lambda e: e.tensor_tensor(out=OT[:, c, tok0:tok0 + 128], in0=bank(nb_)[:, cs],
                                                  in1=rec[:, cs], op=ALU.mult),
                 [psb[nb_], b_rec], [OB[min(grp, 4)]])

        qk(0)
        for u in range(36):
            ex(u)
            if u >= 8 and u % 8 < 4:
                fin_q(u // 8 - 1, u % 8)
            if u + 1 < 36:
                qk(u + 1)
            pv(u)
        fin_q(4, 0)
        fin_q(4, 1)
    if stop <= 3:
        dump_h("o0")
        if debug:
            for nm, src, shp in (("q", qT_s, [D, TT]), ("k", kT_s, [D, TT])):
                dd = nc.dram_tensor("dbg_" + nm, shp, BF16, kind="ExternalOutput").ap()
                P.dma("sp", dd[:, :], src[:, :], reads=[qT_b, kT_b], writes=[Buf("dd" + nm)])
            dd = nc.dram_tensor("dbg_v", [33 * 128, 1024], BF16, kind="ExternalOutput").ap()
            P.dma("sp", dd[:, :], v_s[0:33 * 128 * 1024].rearrange("(t f) -> t f", f=1024), reads=[v_b], writes=[Buf("ddv")])
        return finish()

    def out_proj_residual(wsb, b_wsb, nkc, src, srcB, layer, gkind, blocks, prr_):
        for pc, (wt, wb_) in enumerate(zip(wsb, b_wsb)):
            for j in range(4):
                dc = pc * 4 + j
                for b in blocks:
                    t0, n = BLK[b]
                    t = 0 if b < 4 else 1
                    pb = prr_.next()
                    for kc in range(nkc):
                        mm(bank(pb)[:, :n], wt[:, kc, j * 128:(j + 1) * 128], src[:, kc, t0:t0 + n],
                           kc == 0, kc == nkc - 1, [wb_, srcB[b]], [psb[pb]], kc == nkc - 1)
                    xv = xt_blk(b)
                    P.op("dve", lambda e, pb=pb, n=n, xv=xv, dc=dc, t=t: e.scalar_tensor_tensor(
                        out=xv[:, dc, :], in0=bank(pb)[:, :n], scalar=der[layer][:, gkind, t, dc:dc + 1], in1=xv[:, dc, :],
                        op0=ALU.mult, op1=ALU.add), [psb[pb], b_der, XB[b]], [XB[b]])

    out_proj_residual(wo, b_wo, 8, OT, OB, 0, 2, range(5), RR([0, 1, 2, 3]))
    if stop <= 4:
        dump_x("mix0")
        return finish()

    def moe(layer, nblk):
        P.barrier()
        A = Arena()
        ntile = 18 if nblk == 5 else 16
        norm_mod(A, 0, nblk, 4, 3, layer)
        P.barrier()
        A = Arena()
        lg = A.alloc(18 * 16 * 4).rearrange("p (t e) -> p t e", e=16)
        b_lg = Buf("lg")
        for ti in range(ntile):
            b = min(ti // 4, 4)
            for kc in range(8):
                mm(bank(1)[:, 16 * ti:16 * ti + 16], HT[:, kc, ti * 128:(ti + 1) * 128], rwb[:, kc, :],
                   kc == 0, kc == 7, [HB[b], b_rwb], [psb[1]], (kc == 7 and ti == ntile - 1))
        NE = ntile * 16
        NG = ntile * 4
        sc_ = A.alloc(18 * 16 * 4)
        sel_ = A.alloc(18 * 16 * 4)
        t1_ = A.alloc(18 * 16 * 4)
        t2_ = A.alloc(18 * 16 * 4)
        m1_ = A.alloc(72 * 4)
        m2_ = A.alloc(72 * 4)
        gs_ = A.alloc(72 * 4)
        gm_ = A.alloc(18 * 4)
        ws_ = A.alloc(18 * 4)
        gbf = A.alloc(18 * 16 * 2, BF16)
        gT = A.alloc(TT * 2, BF16)
        selm = A.alloc(16 * 128 * 2, BF16).rearrange("p (e m) -> p e m", e=16)
        b_r = Buf("route")
        b_gbf, b_gT, b_selm = Buf("gbf"), Buf("gT"), Buf("selm")
        P.dma("pool", selm[0:16], sel_d.rearrange("k (e m) -> k e m", e=16), writes=[b_selm])
        g4 = lambda ap: ap[:, :NE].rearrange("p (g k) -> p g k", k=4)
        bc4 = lambda ap: ap[:, :NG].unsqueeze(2).to_broadcast([128, NG, 4])
        P.op("act", lambda e: e.activation(out=sc_[:, :NE], in_=bank(1)[:, :NE], func=AF.Sigmoid), [psb[1]], [b_r])
        P.op("dve", lambda e: e.tensor_tensor(out=sel_[:, :NE], in0=sc_[:, :NE], in1=rb18.rearrange("p t e -> p (t e)")[:, :NE],
                                              op=ALU.add), [b_r, b_rb18], [b_r])
        P.op("dve", lambda e: e.tensor_reduce(out=m1_[:, :NG], in_=g4(sel_), axis=AX.X, op=ALU.max), [b_r], [b_r])
        P.op("dve", lambda e: e.tensor_tensor(out=g4(t1_), in0=g4(sel_), in1=bc4(m1_), op=ALU.is_equal), [b_r], [b_r])
        P.op("dve", lambda e: e.scalar_tensor_tensor(out=t2_[:, :NE], in0=t1_[:, :NE], scalar=-1e9, in1=sel_[:, :NE],
                                                     op0=ALU.mult, op1=ALU.add), [b_r], [b_r])
        P.op("dve", lambda e: e.tensor_reduce(out=m2_[:, :NG], in_=g4(t2_), axis=AX.X, op=ALU.max), [b_r], [b_r])
        P.op("dve", lambda e: e.tensor_tensor(out=gs_[:, :NG], in0=m1_[:, :NG], in1=m2_[:, :NG], op=ALU.add), [b_r], [b_r])
        P.op("dve", lambda e: e.tensor_reduce(out=gm_[:, :ntile], in_=gs_[:, :NG].rearrange("p (t g) -> p t g", g=4),
                                              axis=AX.X, op=ALU.max), [b_r], [b_r])
        P.op("dve", lambda e: e.tensor_tensor(out=m1_[:, :NG].rearrange("p (t g) -> p t g", g=4),
                                              in0=gs_[:, :NG].rearrange("p (t g) -> p t g", g=4),
                                              in1=gm_[:, :ntile].unsqueeze(2).to_broadcast([128, ntile, 4]), op=ALU.is_equal),
             [b_r], [b_r])
        P.op("dve", lambda e: e.tensor_tensor(out=g4(t1_), in0=g4(sel_), in1=bc4(m2_), op=ALU.is_ge), [b_r], [b_r])
        P.op("dve", lambda e: e.tensor_tensor(out=g4(t1_), in0=g4(t1_), in1=bc4(m1_), op=ALU.mult), [b_r], [b_r])
        P.op("dve", lambda e: e.tensor_tensor(out=t2_[:, :NE], in0=t1_[:, :NE], in1=sc_[:, :NE], op=ALU.mult), [b_r], [b_r])
        P.op("dve", lambda e: e.tensor_reduce(out=ws_[:, :ntile], in_=t2_[:, :NE].rearrange("p (t e) -> p t e", e=16),
                                              axis=AX.X, op=ALU.add), [b_r], [b_r])
        P.op("dve", lambda e: e.reciprocal(out=ws_[:, :ntile], in_=ws_[:, :ntile]), [b_r], [b_r])
        P.op("dve", lambda e: e.tensor_tensor(out=gbf[:, :NE].rearrange("p (t e) -> p t e", e=16),
                                              in0=t2_[:, :NE].rearrange("p (t e) -> p t e", e=16),
                                              in1=ws_[:, :ntile].unsqueeze(2).to_broadcast([128, ntile, 16]), op=ALU.mult),
             [b_r], [b_gbf])
        for ti in range(ntile):
            pb = 2 + ti // 8
            o = (ti % 8) * 128
            P.op("pe", lambda e, ti=ti, pb=pb, o=o: e.transpose(bankbf(pb)[0:16, o:o + 128], gbf[:, 16 * ti:16 * ti + 16], ident),
                 [b_gbf, b_ident], [psb[pb]], inc=(ti % 8 == 7 or ti == ntile - 1))
        for g in range((ntile + 7) // 8):
            nn = min(8, ntile - 8 * g) * 128
            P.op("dve", lambda e, g=g, nn=nn: e.tensor_copy(out=gT[0:16, 1024 * g:1024 * g + nn], in_=bankbf(2 + g)[0:16, :nn]),
                 [psb[2 + g]], [b_gT])
        wE = [[A.alloc(8192, BF16) for _ in range(3)] for _ in range(2)]
        b_wE = [[Buf(f"wE{i}{j}") for j in range(3)] for i in range(2)]
        hid = [A.alloc(4 * 512 * 2, BF16).rearrange("p (f n) -> p f n", f=4) for _ in range(2)]
        b_hid = [Buf("hid0"), Buf("hid1")]
        sg = [A.alloc(1024, BF16) for _ in range(2)]
        b_sg = [Buf("sg0"), Buf("sg1")]
        gbc = [A.alloc(1024, BF16) for _ in range(2)]
        b_gbc = [Buf("gbc0"), Buf("gbc1")]
        k_g = RR([1, 2])
        k_u = RR([3, 4])
        k_d = RR([5, 6])
        wts = {}

        def gate_up(ex_, b, hb):
            s = ex_ % 2
            wg_t, wu_t, _ = wts[ex_]
            t0, n = BLK[b]
            mm(bank(7)[:, :n], selm[0:16, ex_, :], gT[0:16, t0:t0 + n], True, True, [b_selm, b_gT], [psb[7]], True)
            P.op("act", lambda e: e.activation(out=gbc[hb][:, :n], in_=bank(7)[:, :n], func=AF.Copy),
                 [psb[7]], [b_gbc[hb]])
            for f in range(4):
                pg, pu = k_g.next(), k_u.next()
                for kc in range(8):
                    mm(bank(pg)[:, :n], wg_t[:, kc, f * 128:(f + 1) * 128], HT[:, kc, t0:t0 + n],
                       kc == 0, kc == 7, [b_wE[s][0], HB[b]], [psb[pg]], kc == 7)
                for kc in range(8):
                    mm(bank(pu)[:, :n], wu_t[:, kc, f * 128:(f + 1) * 128], HT[:, kc, t0:t0 + n],
                       kc == 0, kc == 7, [b_wE[s][1], HB[b]], [psb[pu]], kc == 7)
                si = f % 2
                P.op("act", lambda e: e.activation(out=sg[si][:, :n], in_=bank(pg)[:, :n], func=AF.Silu),
                     [psb[pg]], [b_sg[si]])
                P.op("dve", lambda e: e.tensor_tensor(out=sg[si][:, :n], in0=sg[si][:, :n],
                                                      in1=gbc[hb][:, :n], op=ALU.mult),
                     [b_sg[si], b_gbc[hb]], [b_sg[si]])
                P.op("dve", lambda e: e.tensor_tensor(
                    out=hid[hb][:, f, :n], in0=bank(pu)[:, :n], in1=sg[si][:, :n], op=ALU.mult),
                    [psb[pu], b_sg[si]], [b_hid[hb]])

        def down(ex_, b, hb):
            s = ex_ % 2
            wd_t = wts[ex_][2]
            t0, n = BLK[b]
            t = 0 if b < 4 else 1
            xv = xt_blk(b)
            for dc in range(8):
                pd = k_d.next()
                for f in range(4):
                    mm(bank(pd)[:, :n], wd_t[:, f, dc * 128:(dc + 1) * 128], hid[hb][:, f, :n],
                       f == 0, f == 3, [b_wE[s][2], b_hid[hb]], [psb[pd]], f == 3)
                P.op("dve", lambda e: e.scalar_tensor_tensor(
                    out=xv[:, dc, :], in0=bank(pd)[:, :n], scalar=der[layer][:, 5, t, dc:dc + 1], in1=xv[:, dc, :],
                    op0=ALU.mult, op1=ALU.add), [psb[pd], b_der, XB[b]], [XB[b]])

        prev = None
        cnt = 0
        for ex_ in range(16):
            s = ex_ % 2
            wg_t = wE[s][0].rearrange("p (c n) -> p c n", c=8)
            wu_t = wE[s][1].rearrange("p (c n) -> p c n", c=8)
            wd_t = wE[s][2].rearrange("p (c n) -> p c n", c=4)
            wts[ex_] = (wg_t, wu_t, wd_t)
            P.dma("pool", wg_t, wg_d[layer, ex_].rearrange("(c p) n -> p c n", p=128), writes=[b_wE[s][0]])
            P.dma("pool", wu_t, wu_d[layer, ex_].rearrange("(c p) n -> p c n", p=128), writes=[b_wE[s][1]])
            P.dma("pool", wd_t, wd_d[layer, ex_].rearrange("(c p) n -> p c n", p=128), writes=[b_wE[s][2]])
            for b in range(nblk):
                hb = cnt % 2
                cnt += 1
                gate_up(ex_, b, hb)
                if prev is not None:
                    down(*prev)
                prev = (ex_, b, hb)
        down(*prev)

    moe(0, 5)
    if stop <= 5:
        dump_x("out0")
        return finish()

    P.barrier()
    A = Arena()
    norm_mod(A, 0, 5, 1, 0, 1)
    if stop <= 6:
        dump_h("h1")
        return finish()
    P.barrier()
    A = Arena()
    wpc = [A.alloc(8192, BF16).rearrange("p (c n) -> p c n", c=8) for _ in range(3)]
    b_wpc = [Buf(f"wpc{i}") for i in range(3)]
    wrr = RR(range(3))
    cosT = A.alloc(T * 4)
    sinT = A.alloc(T * 4)
    b_cs = Buf("cossin")
    P.dma("sp", cosT, cos_d[:, :], writes=[b_cs])
    P.dma("sp", sinT, sin_d[:, :], writes=[b_cs])
    ra = [A.alloc(2048) for _ in range(2)]
    rb_ = [A.alloc(2048) for _ in range(2)]
    b_ra = [Buf("ra0"), Buf("ra1")]
    b_rbb = [Buf("rb0"), Buf("rb1")]
    rt2 = [[A.alloc(2048) for _ in range(2)] for _ in range(4)]
    b_rt2 = [[Buf(f"rt{i}_{j}") for j in range(2)] for i in range(4)]
    rows1 = [A.alloc(TT * 2, BF16) for _ in range(2)]
    rows2 = [A.alloc(TT * 2, BF16) for _ in range(2)]
    b_rows1 = [Buf("rows10"), Buf("rows11")]
    b_rows2 = [Buf("rows20"), Buf("rows21")]
    vst = [A.alloc(1024, BF16) for _ in range(4)]
    b_vst = [Buf(f"vst{i}") for i in range(4)]
    vsr = RR(range(4))
    pA = RR([1, 3])
    pB = RR([2, 4])
    prr = RR([5, 6, 7])
    hcount = 0
    for pc in range(4):
        wi = wrr.next()
        load_piece(wpc[wi], b_wpc[wi], ret_w_in_d[:, pc * 512:(pc + 1) * 512])
        isq = pc < 2
        dst_s, dst_b = (qT_s, qT_b) if isq else (kT_s, kT_b)
        for hh in range(2):
            hd = (pc % 2) * 2 + hh
            ri = hcount % 2
            hcount += 1
            nb_ = 4 if isq else 5
            for b in range(nb_):
                t0, n = BLK[b]
                p1, p2 = pA.next(), pB.next()
                for half, pb in ((0, p1), (1, p2)):
                    j = hh * 2 + half
                    for kc in range(8):
                        mm(bank(pb)[:, :n], wpc[wi][:, kc, j * 128:(j + 1) * 128], HT[:, kc, t0:t0 + n],
                           kc == 0, kc == 7, [b_wpc[wi], HB[b]], [psb[pb]], kc == 7)
                if b < 4:
                    ai = b % 2
                    P.op("act", lambda e, ai=ai, p1=p1: e.activation(out=ra[ai], in_=bank(p1), func=AF.Copy), [psb[p1]], [b_ra[ai]])
                    P.op("act", lambda e, ai=ai, p2=p2: e.activation(out=rb_[ai], in_=bank(p2), func=AF.Copy), [psb[p2]], [b_rbb[ai]])
                    cs, sn = cosT[:, t0:t0 + 512], sinT[:, t0:t0 + 512]
                    P.op("dve", lambda e, ai=ai, cs=cs: e.tensor_tensor(out=rt2[0][ai], in0=ra[ai], in1=cs, op=ALU.mult), [b_ra[ai], b_cs], [b_rt2[0][ai]])
                    P.op("dve", lambda e, ai=ai, sn=sn: e.tensor_tensor(out=rt2[1][ai], in0=rb_[ai], in1=sn, op=ALU.mult), [b_rbb[ai], b_cs], [b_rt2[1][ai]])
                    P.op("dve", lambda e, ri=ri, t0=t0: e.tensor_tensor(out=rows1[ri][:, t0:t0 + 512], in0=rt2[0][ai], in1=rt2[1][ai], op=ALU.subtract),
                         [b_rt2[0][ai], b_rt2[1][ai]], [b_rows1[ri]])
                    P.op("pool", lambda e, ai=ai, sn=sn: e.tensor_tensor(out=rt2[2][ai], in0=ra[ai], in1=sn, op=ALU.mult), [b_ra[ai], b_cs], [b_rt2[2][ai]])
                    P.op("pool", lambda e, ai=ai, cs=cs: e.tensor_tensor(out=rt2[3][ai], in0=rb_[ai], in1=cs, op=ALU.mult), [b_rbb[ai], b_cs], [b_rt2[3][ai]])
                    P.op("dve", lambda e, ri=ri, t0=t0: e.tensor_tensor(out=rows2[ri][:, t0:t0 + 512], in0=rt2[2][ai], in1=rt2[3][ai], op=ALU.add),
                         [b_rt2[2][ai], b_rt2[3][ai]], [b_rows2[ri]])
                else:
                    P.op("act", lambda e, ri=ri, p1=p1, t0=t0, n=n: e.activation(out=rows1[ri][:, t0:t0 + n], in_=bank(p1)[:, :n], func=AF.Copy),
                         [psb[p1]], [b_rows1[ri]])
                    P.op("act", lambda e, ri=ri, p2=p2, t0=t0, n=n: e.activation(out=rows2[ri][:, t0:t0 + n], in_=bank(p2)[:, :n], func=AF.Copy),
                         [psb[p2]], [b_rows2[ri]])
            ncol = T if isq else TT
            P.dma("sp", dst_s[(2 * hd) * 128:(2 * hd + 1) * 128, 0:ncol], rows1[ri][:, 0:ncol], reads=[b_rows1[ri]], writes=[dst_b])
            P.dma("sp", dst_s[(2 * hd + 1) * 128:(2 * hd + 2) * 128, 0:ncol], rows2[ri][:, 0:ncol], reads=[b_rows2[ri]], writes=[dst_b])
    v2 = v_s.rearrange("(t f) -> t f", f=2 * D)
    for pc in range(4, 12):
        wi = wrr.next()
        load_piece(wpc[wi], b_wpc[wi], ret_w_in_d[:, pc * 512:(pc + 1) * 512])
        isv = pc < 8
        hd = (pc - 4) % 4
        for ti in range(18 if isv else 16):
            pb = prr.next()
            b = min(ti // 4, 4)
            for kc in range(8):
                mm(bank(pb), HT[:, kc, ti * 128:(ti + 1) * 128], wpc[wi][:, kc, :], kc == 0, kc == 7,
                   [b_wpc[wi], HB[b]], [psb[pb]], kc == 7)
            vi = vsr.next()
            if isv:
                copy_any(evac_rr.next(), vst[vi], bank(pb), [psb[pb]], [b_vst[vi]])
                P.dma("sp", v2[ti * 128:(ti + 1) * 128, hd * 512:(hd + 1) * 512], vst[vi], reads=[b_vst[vi]], writes=[v_b])
            else:
                P.op("act", lambda e, vi=vi, pb=pb: e.activation(out=vst[vi], in_=bank(pb), func=AF.Silu), [psb[pb]], [b_vst[vi]])
                P.dma("sp", sg_s[ti * 128:(ti + 1) * 128, hd * 512:(hd + 1) * 512], vst[vi], reads=[b_vst[vi]], writes=[sg_b])

    if stop <= 6.5:
        return finish()
    P.barrier()
    A = Arena()
    rc = A.alloc(772 * 4)
    b_rc = Buf("rc")
    P.dma("sp", rc, rcst_d[:, :], writes=[b_rc])
    lgt = A.alloc(32)
    b_lg = Buf("lgt")
    P.dma("sp", lgt, bass.AP(ret_dec_h, 0, [[0, 128], [1, 8]]), writes=[b_lg])
    P.op("act", lambda e: e.activation(out=lgt, in_=lgt, func=AF.Exp, scale=-1.0), [b_lg], [b_lg])
    P.op("act", lambda e: e.activation(out=lgt, in_=lgt, func=AF.Ln, bias=1.0), [b_lg], [b_lg])
    P.op("dve", lambda e: e.tensor_scalar(out=lgt, in0=lgt, scalar1=-1.0, scalar2=None, op0=ALU.mult), [b_lg], [b_lg])
    dec = A.alloc(4 * 128 * 4).rearrange("p (h n) -> p h n", h=4)
    rowF = A.alloc(4 * 128 * 4).rearrange("p (h n) -> p h n", h=4)
    rowB = A.alloc(4 * 128 * 4).rearrange("p (h n) -> p h n", h=4)
    colv = A.alloc(4 * 4 * 4).rearrange("p (h k) -> p h k", h=4)
    e1 = A.alloc(512)
    e2 = A.alloc(512)
    b_dec, b_e = Buf("dec"), Buf("e12")
    for hd in range(4):
        lf, lb = lgt[:, hd:hd + 1], lgt[:, 4 + hd:5 + hd]
        P.op("act", lambda e, lf=lf: e.activation(out=e1, in_=rc[:, 0:128], func=AF.Exp, scale=lf), [b_rc, b_lg], [b_e])
        P.op("act", lambda e, lb=lb: e.activation(out=e2, in_=rc[:, 128:256], func=AF.Exp, scale=lb), [b_rc, b_lg], [b_e])
        P.op("dve", lambda e: e.tensor_tensor(out=e1, in0=e1, in1=rc[:, 256:384], op=ALU.mult), [b_e, b_rc], [b_e])
        P.op("dve", lambda e: e.tensor_tensor(out=e2, in0=e2, in1=rc[:, 384:512], op=ALU.mult), [b_e, b_rc], [b_e])
        P.op("dve", lambda e, hd=hd: e.tensor_tensor(out=dec[:, hd, :], in0=e1, in1=e2, op=ALU.add), [b_e], [b_dec])
        P.op("act", lambda e, hd=hd, lf=lf: e.activation(out=rowF[:, hd, :], in_=rc[:, 512:640], func=AF.Exp, scale=lf), [b_rc, b_lg], [b_dec])
        P.op("act", lambda e, hd=hd, lb=lb: e.activation(out=rowB[:, hd, :], in_=rc[:, 640:768], func=AF.Exp, scale=lb), [b_rc, b_lg], [b_dec])
        P.op("act", lambda e, hd=hd, lf=lf: e.activation(out=colv[:, hd, 0:1], in_=rc[:, 768:769], func=AF.Exp, scale=lf), [b_rc, b_lg], [b_dec])
        P.op("act", lambda e, hd=hd, lb=lb: e.activation(out=colv[:, hd, 1:2], in_=rc[:, 769:770], func=AF.Exp, scale=lb), [b_rc, b_lg], [b_dec])
        P.op("act", lambda e, hd=hd, lf=lf: e.activation(out=colv[:, hd, 2:3], in_=rc[:, 770:771], func=AF.Exp, scale=lf), [b_rc, b_lg], [b_dec])
        P.op("act", lambda e, hd=hd, lb=lb: e.activation(out=colv[:, hd, 3:4], in_=rc[:, 770:771], func=AF.Exp, scale=lb), [b_rc, b_lg], [b_dec])
        P.op("dve", lambda e, hd=hd: e.tensor_scalar(out=colv[:, hd, 0:2], in0=colv[:, hd, 0:2], scalar1=1.0 / 16.0, scalar2=None, op0=ALU.mult),
             [b_dec], [b_dec])

    Qh = A.alloc(2 * T * 2, BF16).rearrange("p (c n) -> p c n", c=2)
    QF = A.alloc(2 * T * 2, BF16).rearrange("p (c n) -> p c n", c=2)
    QB = A.alloc(2 * T * 2, BF16).rearrange("p (c n) -> p c n", c=2)
    Kh = A.alloc(2 * TT * 2, BF16).rearrange("p (c n) -> p c n", c=2)
    zT = A.alloc(4 * T * 2, BF16).rearrange("p (c n) -> p c n", c=4)
    woh = A.alloc(4 * 1024 * 2, BF16).rearrange("p (c n) -> p c n", c=4)
    gnh = A.alloc(2048)
    b_Qh, b_QF, b_Kh, b_woh, b_gn = Buf("Qh"), Buf("QF"), Buf("Kh"), Buf("woh"), Buf("gn")
    b_zT = [Buf(f"zT{i}") for i in range(4)]
    b_QFc = [Buf("QFc0"), Buf("QFc1")]
    Vt = [A.alloc(1024, BF16) for _ in range(3)]
    b_Vt = [Buf(f"Vt{i}") for i in range(3)]
    SGt = [A.alloc(1024, BF16) for _ in range(2)]
    b_SGt = [Buf("SGt0"), Buf("SGt1")]
    Sst = [A.alloc(2 * 512 * 4).rearrange("p (c n) -> p c n", c=2) for _ in range(2)]
    b_Sst = [Buf("SstF"), Buf("SstB")]
    Sfb = [A.alloc(2 * 512 * 2, BF16).rearrange("p (c n) -> p c n", c=2) for _ in range(2)]
    b_Sfb = [Buf("Sfb0"), Buf("Sfb1")]
    Ksc = [A.alloc(512, BF16) for _ in range(2)]
    b_Ksc = [Buf("Ksc0"), Buf("Ksc1")]
    Ap = [A.alloc(256, BF16) for _ in range(2)]
    b_Ap = [Buf("Ap0"), Buf("Ap1")]
    z1 = [view(HT_raw_off + 32768 + 2048 * i, 2048) for i in range(2)]
    b_z1 = [Buf("z10"), Buf("z11")]
    zb = [A.alloc(1024, BF16) for _ in range(2)]
    b_zb = [Buf("zb0"), Buf("zb1")]
    junk = A.alloc(1024, BF16)
    b_junk = Buf("junk")
    ssq = A.alloc(32)
    b_ssq = Buf("ssq")
    SbS = view(HT_raw_off, 16 * 2 * 512 * 2, BF16).rearrange("p (c h n) -> p c h n", c=16, h=2)
    b_SbS = [Buf(f"SbS{i}") for i in range(16)]
    vrr = RR(range(3))
    oprr = RR([7, 3, 6])

    def kv_part1(hd, tt, direction, ki):
        vi = vrr.next()
        P.dma("sp", Vt[vi], v2[tt * 128:(tt + 1) * 128, hd * 512:(hd + 1) * 512], reads=[v_b], writes=[b_Vt[vi]])
        c0 = tt * 128
        for half in range(2):
            P.op("pe", lambda e: e.transpose(bankbf(0)[:, half * 128:(half + 1) * 128], Kh[:, half, c0:c0 + 128], ident),
                 [b_Kh, b_ident], [psb[0]], inc=(half == 1))
        copy_any("act" if direction == 0 else "dve", Ksc[ki], bankbf(0)[:, 0:256], [psb[0], b_dec], [b_Ksc[ki]],
                 scale=colv[:, hd, direction:direction + 1])
        return vi

    def kv_part2(hd, direction, first, ki, vi, banks, store_bf=None, store_buf=None):
        for half in range(2):
            pb = banks[half]
            mm(bank(pb), Ksc[ki][:, half * 128:(half + 1) * 128], Vt[vi], True, True, [b_Ksc[ki], b_Vt[vi]], [psb[pb]], True)
            if first:
                P.op("dve", lambda e: e.tensor_copy(out=Sst[direction][:, half, :], in_=bank(pb)),
                     [psb[pb]], [b_Sst[direction]])
            else:
                P.op("dve", lambda e: e.scalar_tensor_tensor(
                    out=Sst[direction][:, half, :], in0=Sst[direction][:, half, :], scalar=colv[:, hd, 2 + direction:3 + direction],
                    in1=bank(pb), op0=ALU.mult, op1=ALU.add), [psb[pb], b_dec, b_Sst[direction]], [b_Sst[direction]])
        if store_bf is not None:
            P.op("act", lambda e: e.activation(out=store_bf, in_=Sst[direction], func=AF.Copy), [b_Sst[direction]], [store_buf])

    def ztrans(c, si):
        c0 = c * 128
        for j in range(4):
            P.op("pe", lambda e: e.transpose(bankbf(6)[:, j * 128:(j + 1) * 128], zb[si][:, j * 128:(j + 1) * 128], ident),
                 [b_zb[si], b_ident], [psb[6]], inc=(j == 3))
        P.op("act", lambda e: e.activation(out=zT[:, :, c0:c0 + 128],
                                           in_=bankbf(6)[:, 0:512].rearrange("p (j n) -> p j n", j=4), func=AF.Copy),
             [psb[6]], [b_zT[c // 4]])

    pending_op = []
    for hd in range(4):
        P.dma("sp", Qh, qT_s[(2 * hd) * 128:(2 * hd + 2) * 128, 0:T].rearrange("(c p) n -> p c n", p=128), reads=[qT_b], writes=[b_Qh])
        P.dma("sp", Kh, kT_s[(2 * hd) * 128:(2 * hd + 2) * 128, :].rearrange("(c p) n -> p c n", p=128), reads=[kT_b], writes=[b_Kh])
        P.dma("sp", gnh, bass.AP(ret_norm_h, hd * 512, [[0, 128], [1, 512]]), writes=[b_gn])
        bw = [(17, True, None), (16, False, 15)] + [(c, False, c - 1) for c in range(15, 0, -1)]
        pend = None
        for i, (tt, first, st) in enumerate(bw):
            ki = i % 2
            vi = kv_part1(hd, tt, 1, ki)
            if pend is not None:
                kv_part2(*pend)
            banks = (1, 2) if i % 2 == 0 else (4, 5)
            pend = (hd, 1, first, ki, vi, banks) + ((SbS[:, st, :, :], b_SbS[st]) if st is not None else (None, None))
            for _ in range(2):
                if pending_op:
                    pending_op.pop(0)()
        kv_part2(*pend)
        while pending_op:
            pending_op.pop(0)()
        P.dma("pool", woh, ret_w_out_d[hd * 512:(hd + 1) * 512, :].rearrange("(c p) n -> p c n", p=128), writes=[b_woh])
        vi = kv_part1(hd, 16, 0, 0)
        kv_part2(hd, 0, True, 0, vi, (1, 2))
        vi = kv_part1(hd, 17, 0, 0)
        kv_part2(hd, 0, False, 0, vi, (1, 2), Sfb[0], b_Sfb[0])
        prevz = None
        for c in range(16):
            si = c % 2
            c0 = c * 128
            if c < 15:
                vi = kv_part1(hd, c, 0, 0)
            else:
                vi = vrr.next()
                P.dma("sp", Vt[vi], v2[c * 128:(c + 1) * 128, hd * 512:(hd + 1) * 512], reads=[v_b], writes=[b_Vt[vi]])
            P.dma("sp", SGt[si], sg_s[c * 128:(c + 1) * 128, hd * 512:(hd + 1) * 512], reads=[sg_b], writes=[b_SGt[si]])
            P.op("dve", lambda e: e.tensor_tensor(out=QF[:, :, c0:c0 + 128], in0=Qh[:, :, c0:c0 + 128],
                                                  in1=rowF[:, hd, :].unsqueeze(1).to_broadcast([128, 2, 128]), op=ALU.mult),
                 [b_Qh, b_dec], [b_QFc[si]])
            P.op("dve", lambda e: e.tensor_tensor(out=QB[:, :, c0:c0 + 128], in0=Qh[:, :, c0:c0 + 128],
                                                  in1=rowB[:, hd, :].unsqueeze(1).to_broadcast([128, 2, 128]), op=ALU.mult),
                 [b_Qh, b_dec], [b_QFc[si]])
            for half in range(2):
                mm(bank(3)[:, 0:128], Kh[:, half, c0:c0 + 128], Qh[:, half, c0:c0 + 128], half == 0, half == 1,
                   [b_Kh, b_Qh], [psb[3]], half == 1)
            P.op("dve", lambda e: e.tensor_tensor(out=Ap[si], in0=bank(3)[:, 0:128], in1=dec[:, hd, :], op=ALU.mult),
                 [psb[3], b_dec], [b_Ap[si]])
            if c < 15:
                kv_part2(hd, 0, False, 0, vi, (1, 2), Sfb[1 - si], b_Sfb[1 - si])
            po = 4 + si
            mm(bank(po), Ap[si], Vt[vi], True, False, [b_Ap[si], b_Vt[vi]], [psb[po]], False)
            for half in range(2):
                mm(bank(po), QF[:, half, c0:c0 + 128], Sfb[si][:, half, :], False, False, [b_QFc[si], b_Sfb[si]], [psb[po]], False)
            for half in range(2):
                mm(bank(po), QB[:, half, c0:c0 + 128], SbS[:, c, half, :], False, half == 1, [b_QFc[si], b_SbS[c]], [psb[po]], half == 1)
            P.op("dve", lambda e: e.memset(ssq[:, 0:1], 0.0), [], [b_ssq])
            P.op("act", lambda e: e.activation(out=junk, in_=bank(po), func=AF.Square, accum_out=ssq[:, 0:1]),
                 [psb[po]], [b_junk, b_ssq])
            P.op("act", lambda e: e.activation(out=ssq[:, 1:2], in_=ssq[:, 0:1], func=AF.Sqrt, scale=1.0 / 512.0, bias=epsc[:, 1:2]),
                 [b_ssq, b_eps], [b_ssq])
            P.op("dve", lambda e: e.reciprocal(out=ssq[:, 2:3], in_=ssq[:, 1:2]), [b_ssq], [b_ssq])
            P.op("dve", lambda e: e.scalar_tensor_tensor(out=z1[si], in0=bank(po), scalar=ssq[:, 2:3], in1=gnh,
                                                         op0=ALU.mult, op1=ALU.mult),
                 [psb[po], b_ssq, b_gn], [b_z1[si]])
            P.op("pool", lambda e: e.tensor_tensor(out=zb[si], in0=z1[si], in1=SGt[si], op=ALU.mult),
                 [b_z1[si], b_SGt[si]], [b_zb[si]])
            if prevz is not None:
                ztrans(*prevz)
            prevz = (c, si)
        ztrans(*prevz)
        def op_group(dc, b):
            t0, n = BLK[b]
            pb = oprr.next()
            for j in range(4):
                mm(bank(pb), woh[:, j, dc * 128:(dc + 1) * 128], zT[:, j, t0:t0 + 512], j == 0, j == 3,
                   [b_woh, b_zT[b]], [psb[pb]], j == 3)
            P.op("dve", lambda e: e.scalar_tensor_tensor(
                out=XT[:, dc, t0:t0 + 512], in0=bank(pb), scalar=der[1][:, 2, 0, dc:dc + 1], in1=XT[:, dc, t0:t0 + 512],
                op0=ALU.mult, op1=ALU.add), [psb[pb], b_der, XB[b]], [XB[b]])
        pending_op = [(lambda dc=dc, b=b: op_group(dc, b)) for dc in range(8) for b in range(4)]
    while pending_op:
        pending_op.pop(0)()
    if stop <= 7:
        dump_x("mix1")
        return finish()

    moe(1, 4)
    if stop <= 8:
        dump_x("out1")
        return finish()

    P.barrier()
    A = Arena()
    sq = [A.alloc(8 * 512 * 2, BF16).rearrange("p (c n) -> p c n", c=8) for _ in range(2)]
    rst = [A.alloc(2048) for _ in range(2)]
    yt = [A.alloc(2048) for _ in range(4)]
    hiT = [A.alloc(8 * 512 * 2, BF16).rearrange("p (c n) -> p c n", c=8) for _ in range(2)]
    loT = [A.alloc(8 * 512 * 2, BF16).rearrange("p (c n) -> p c n", c=8) for _ in range(2)]
    o1 = [A.alloc(4096) for _ in range(2)]
    o2 = [A.alloc(4096) for _ in range(2)]
    b_sq = [[Buf(f"fsq{i}_{c}") for c in range(8)] for i in range(2)]
    b_rst = [Buf("frst0"), Buf("frst1")]
    b_hi = [Buf("fhi0"), Buf("fhi1")]
    b_lo = [Buf("flo0"), Buf("flo1")]
    b_yt = [Buf(f"fyt{i}") for i in range(4)]
    b_o1 = [Buf("fo10"), Buf("fo11")]
    b_o2 = [Buf("fo20"), Buf("fo21")]
    kk = [0, 0]

    def f_stats(b):
        t0 = b * 512
        i = b % 2
        for c in range(8):
            P.op("act", lambda e: e.activation(out=sq[i][:, c, :], in_=XT[:, c, t0:t0 + 512], func=AF.Square), [XB[b]], [b_sq[i][c]])
            mm(bank(0), ones, sq[i][:, c, :], c == 0, c == 7, [b_ones, b_sq[i][c]], [psb[0]], c == 7)
        P.op("act", lambda e: e.activation(out=rst[i], in_=bank(0), func=AF.Sqrt, bias=epsc[:, 0:1]), [psb[0], b_eps], [b_rst[i]])

    def f_recip(b):
        i = b % 2
        P.op("dve", lambda e: e.reciprocal(out=rst[i], in_=rst[i]), [b_rst[i]], [b_rst[i]])

    def f_split(b):
        t0 = b * 512
        i = b % 2
        for c in range(8):
            yi = kk[0] % 4
            kk[0] += 1
            P.op("dve", lambda e: e.scalar_tensor_tensor(
                out=yt[yi], in0=XT[:, c, t0:t0 + 512], scalar=nS[:, 32 + c:33 + c], in1=rst[i], op0=ALU.mult, op1=ALU.mult),
                [XB[b], b_nS, b_rst[i]], [b_yt[yi]])
            P.op("act", lambda e: e.activation(out=hiT[i][:, c, :], in_=yt[yi], func=AF.Copy), [b_yt[yi]], [b_hi[i]])
            P.op("pool", lambda e: e.tensor_tensor(out=loT[i][:, c, :], in0=yt[yi], in1=hiT[i][:, c, :], op=ALU.subtract),
                 [b_yt[yi], b_hi[i]], [b_lo[i]])

    def f_emit(b):
        t0 = b * 512
        i = b % 2
        for s4 in range(4):
            oi = kk[1] % 2
            kk[1] += 1
            bh, bl = 1 + 2 * oi, 2 + 2 * oi
            for c in range(8):
                P.op("pe", lambda e: e.transpose(bankbf(bh)[:, c * 128:(c + 1) * 128],
                                                 hiT[i][:, c, s4 * 128:(s4 + 1) * 128], ident),
                     [b_hi[i], b_ident], [psb[bh]], inc=(c == 7))
            for c in range(8):
                P.op("pe", lambda e: e.transpose(bankbf(bl)[:, c * 128:(c + 1) * 128],
                                                 loT[i][:, c, s4 * 128:(s4 + 1) * 128], ident),
                     [b_lo[i], b_ident], [psb[bl]], inc=(c == 7))
            P.op("act", lambda e: e.activation(out=o1[oi], in_=bankbf(bh), func=AF.Copy), [psb[bh]], [b_o1[oi]])
            P.op("dve", lambda e: e.tensor_tensor(out=o2[oi], in0=o1[oi], in1=bankbf(bl), op=ALU.add),
                 [b_o1[oi], psb[bl]], [b_o2[oi]])
            r0_ = t0 + s4 * 128
            P.dma("sp", out_d[r0_:r0_ + 128, :], o2[oi], reads=[b_o2[oi]], writes=[out_b])

    f_stats(0)
    f_recip(0)
    for b in range(4):
        if b + 1 < 4:
            f_stats(b + 1)
        f_split(b)
        if b + 1 < 4:
            f_recip(b + 1)
        f_emit(b)
    return finish()


_CACHE = {}


def _prep_inputs(inp):
    f32 = np.float32
    g = lambda k: np.asarray(inp[k], dtype=f32)
    x, c, ctx, c_ctx = g("x"), g("c"), g("ctx"), g("c_ctx")
    w_ada, b_ada = g("w_ada"), g("b_ada")
    shared = {
        "w_ada": np.ascontiguousarray(w_ada),
        "na_w_in": np.ascontiguousarray(g("na_w_in")[0]),
        "na_w_out": np.ascontiguousarray(g("na_w_out")[0]),
        "natab": _na_table(g("na_rpb")[0]),
        "ret_w_in": np.ascontiguousarray(g("ret_w_in")[0]),
        "ret_w_out": np.ascontiguousarray(g("ret_w_out")[0]),
        "ret_dec": np.concatenate([g("ret_decay_fwd")[0], g("ret_decay_bwd")[0]])[None, :].astype(f32),
        "ret_norm": np.ascontiguousarray(g("ret_norm")),
        "rcst": _ret_consts(),
        "router_w": np.ascontiguousarray(g("router_w")),
        "router_b": g("router_b")[None, :].copy(),
        "moe_w_gate": np.ascontiguousarray(g("moe_w_gate")),
        "moe_w_up": np.ascontiguousarray(g("moe_w_up")),
        "moe_w_down": np.ascontiguousarray(g("moe_w_down")),
        "ident": np.eye(128, dtype=f32),
        "sel": np.ascontiguousarray(np.repeat(np.eye(16, dtype=f32)[:, :, None], 128, axis=2).reshape(16, 16 * 128)),
    }
    cs, sn = _rot_tables()
    shared["rcos"], shared["rsin"] = cs, sn
    maps = []
    for b in range(8):
        vecs = np.zeros((128, NV), f32)
        cv = np.stack([_fm(c[b]), _fm(c_ctx)], axis=-1)
        vecs[:, V_CV:V_CV + 16] = cv.reshape(128, 16)
        for i in range(2):
            vecs[:, V_BADA + 48 * i:V_BADA + 48 * (i + 1)] = _fm(b_ada[i])
            vecs[:, V_NMIX + 8 * i:V_NMIX + 8 * (i + 1)] = _fm(g("norm_mix")[i])
            vecs[:, V_NFFN + 8 * i:V_NFFN + 8 * (i + 1)] = _fm(g("norm_ffn")[i])
        vecs[:, V_FN:V_FN + 8] = _fm(g("final_norm"))
        m = dict(shared)
        m["x"] = np.ascontiguousarray(x[b])
        m["ctx"] = np.ascontiguousarray(ctx[b])
        m["vecs"] = vecs
        maps.append(m)
    return maps


def kernel(**inputs):
    if "nc" not in _CACHE:
        _CACHE["nc"] = build()[0]
    nc = _CACHE["nc"]
    maps = _prep_inputs(inputs)
    res = run_bass_kernel_spmd(nc, maps, core_ids=list(range(8)))
    return np.stack([np.asarray(r["out"], dtype=np.float32) for r in res.results], axis=0)
```
